# Optimizing a Trainium2 kernel written in Bass

```python
import math, functools
import jax, jax.numpy as jnp
from jax import lax
import numpy as np

D_MODEL = 1024
BATCH = 16
SEQ = 4096
DEPTH = 2

GRID_W = 64
CTX_LEN = 256
EPS = 1e-6
N_MOD = 6

SSD_HEADDIM = 64
D_SSD = D_MODEL
SSD_HEADS = D_SSD // SSD_HEADDIM
SSD_GROUPS = 2
SSD_HPG = SSD_HEADS // SSD_GROUPS
SSD_STATE = 128
SSD_GN = SSD_GROUPS * SSD_STATE
D_XBC = D_SSD + 2 * SSD_GN
SSD_CHUNK = 128

CONV_K = 4
CONV_PAD_L = 2

D_LRU = D_MODEL
LRU_BW = 64
LRU_BLOCKS = D_LRU // LRU_BW
RGLRU_C = 8.0

D_MIX = D_SSD + D_LRU
OFF_XBC = D_SSD
OFF_DT = OFF_XBC + D_XBC
OFF_LRU_X = OFF_DT + 2 * SSD_HEADS
OFF_LRU_G = OFF_LRU_X + D_LRU
D_IN = OFF_LRU_G + D_LRU

D_FF = ((8 * D_MODEL // 3 + 127) // 128) * 128
N_EXPERTS = 8
TOP_K = 2
N_DENSE = (DEPTH + 1) // 2
N_MOE = DEPTH // 2

kernel_name = "hybrid_ssd_rglru_moe_dit_prefix"


def rmsnorm(x, w):
    xf = x.astype(jnp.float32)
    y = xf * lax.rsqrt(jnp.mean(jnp.square(xf), axis=-1, keepdims=True) + EPS)
    return (y * w.astype(jnp.float32)).astype(x.dtype)


def modulate(h, shift, scale):
    return h * (1 + scale) + shift


def dwconv(x, w, b):
    ch = x.shape[-1]
    y = lax.conv_general_dilated(x, w[:, None, :].astype(x.dtype), window_strides=(1,),
                                 padding=[(CONV_PAD_L, CONV_K - 1 - CONV_PAD_L)],
                                 dimension_numbers=("NWC", "WIO", "NWC"), feature_group_count=ch)
    return y + b


def segsum(a):
    t = a.shape[-1]
    cs = jnp.cumsum(a, axis=-1)
    diff = cs[..., :, None] - cs[..., None, :]
    return jnp.where(jnp.tril(jnp.ones((t, t), dtype=bool)), diff, -jnp.inf)


def ssd_chunked(X, dA, Bm, Cm, init):
    b, t = X.shape[:2]
    nc = t // SSD_CHUNK
    Xc = X.reshape(b, nc, SSD_CHUNK, SSD_GROUPS, SSD_HPG, SSD_HEADDIM)
    Bc = Bm.reshape(b, nc, SSD_CHUNK, SSD_GROUPS, SSD_STATE)
    Cc = Cm.reshape(b, nc, SSD_CHUNK, SSD_GROUPS, SSD_STATE)
    Ac = dA.reshape(b, nc, SSD_CHUNK, SSD_GROUPS, SSD_HPG).transpose(0, 3, 4, 1, 2)
    A_cs = jnp.cumsum(Ac, axis=-1)
    L = jnp.exp(segsum(Ac))
    CB = jnp.einsum("bclgn,bcsgn->bgcls", Cc, Bc)
    Y_diag = jnp.einsum("bgrcls,bcsgrp->bclgrp", CB[:, :, None] * L, Xc)
    decay_states = jnp.exp(A_cs[..., -1:] - A_cs)
    Xd = Xc * decay_states.transpose(0, 3, 4, 1, 2)[..., None]
    states = jnp.einsum("bclgn,bclgrp->bcgrpn", Bc, Xd)
    states = jnp.concatenate([init[:, None], states], axis=1)
    chunk_tot = jnp.pad(A_cs[..., -1], ((0, 0), (0, 0), (0, 0), (1, 0)))
    decay_chunk = jnp.exp(segsum(chunk_tot))
    new_states = jnp.einsum("bgrzc,bcgrpn->bzgrpn", decay_chunk, states)
    states_in, final = new_states[:, :-1], new_states[:, -1]
    Y_off = jnp.einsum("bclgn,bcgrpn->bclgrp", Cc, states_in) * jnp.exp(A_cs).transpose(0, 3, 4, 1, 2)[..., None]
    return (Y_diag + Y_off).reshape(b, t, SSD_GROUPS, SSD_HPG, SSD_HEADDIM), final


def ssd_branch(z, xbc_raw, dt_raw, conv_w, conv_b, dt_bias, a_log, d_skip, norm_w, init_f, init_b):
    f32 = jnp.float32
    bsz, t, _ = z.shape
    xbc = jax.nn.silu(dwconv(xbc_raw, conv_w, conv_b))
    xf = xbc[..., :D_SSD].reshape(bsz, t, SSD_GROUPS, SSD_HPG, SSD_HEADDIM).astype(f32)
    Bm = xbc[..., D_SSD:D_SSD + SSD_GN].reshape(bsz, t, SSD_GROUPS, SSD_STATE).astype(f32)
    Cm = xbc[..., D_SSD + SSD_GN:].reshape(bsz, t, SSD_GROUPS, SSD_STATE).astype(f32)
    dt = jax.nn.softplus(dt_raw.astype(f32).reshape(bsz, t, 2, SSD_GROUPS, SSD_HPG)
                         + dt_bias.astype(f32).reshape(2, SSD_GROUPS, SSD_HPG))
    A = -jnp.exp(a_log.astype(f32)).reshape(2, SSD_GROUPS, SSD_HPG)
    flip = lambda v: jnp.flip(v, axis=1)
    y_f, s_f = ssd_chunked(xf * dt[:, :, 0, ..., None], dt[:, :, 0] * A[0], Bm, Cm, init_f)
    y_b, s_b = ssd_chunked(flip(xf * dt[:, :, 1, ..., None]), flip(dt[:, :, 1] * A[1]), flip(Bm), flip(Cm), init_b)
    y = y_f + flip(y_b) + d_skip.astype(f32).reshape(SSD_GROUPS, SSD_HPG, 1) * xf
    y = y.reshape(bsz, t, D_SSD).astype(z.dtype)
    return rmsnorm(y * jax.nn.silu(z), norm_w), s_f, s_b


def linear_combine(e1, e2):
    a1, b1 = e1
    a2, b2 = e2
    return a1 * a2, a2 * b1 + b2


def rglru_dir(xc, rw, rb, iw, ib, lam, h0):
    f32 = jnp.float32
    bsz, t, w = xc.shape
    xb = xc.reshape(bsz, t, LRU_BLOCKS, LRU_BW)
    r = jax.nn.sigmoid((jnp.einsum("btnk,nkj->btnj", xb, rw).reshape(bsz, t, w) + rb).astype(f32))
    i = jax.nn.sigmoid((jnp.einsum("btnk,nkj->btnj", xb, iw).reshape(bsz, t, w) + ib).astype(f32))
    log_a = -RGLRU_C * r * jax.nn.softplus(-lam.astype(f32))
    a = jnp.exp(log_a)
    b_in = jnp.sqrt(-jnp.expm1(2.0 * log_a)) * i * xc.astype(f32)
    b_in = b_in.at[:, 0].add(a[:, 0] * h0)
    _, h = lax.associative_scan(linear_combine, (a, b_in), axis=1)
    return h, h[:, -1]


def rglru_bidir(xc, rw, rb, iw, ib, lam, h0_f, h0_b):
    h_f, s_f = rglru_dir(xc, rw[0], rb[0], iw[0], ib[0], lam[0], h0_f)
    h_b, s_b = rglru_dir(jnp.flip(xc, axis=1), rw[1], rb[1], iw[1], ib[1], lam[1], h0_b)
    return h_f + jnp.flip(h_b, axis=1), s_f, s_b


def hybrid_mixer(u_lat, u_ctx, rows, ssd_conv_w, ssd_conv_b, ssd_dt_bias, ssd_a_log, ssd_d, ssd_norm_w,
                 lru_conv_w, lru_conv_b, lru_rw, lru_rb, lru_iw, lru_ib, lru_lambda, lru_norm_w):
    f32 = jnp.float32
    bsz, t, _ = u_lat.shape
    bc = u_ctx.shape[0]

    def parts(u):
        return (u[..., :OFF_XBC], u[..., OFF_XBC:OFF_DT], u[..., OFF_DT:OFF_LRU_X],
                u[..., OFF_LRU_X:OFF_LRU_G], u[..., OFF_LRU_G:])

    zc, xbcc, dtc, lxc, lgc = parts(u_ctx)
    zl, xbcl, dtl, lxl, lgl = parts(u_lat)
    ssd_p = (ssd_conv_w, ssd_conv_b, ssd_dt_bias, ssd_a_log, ssd_d, ssd_norm_w)
    lru_p = (lru_rw, lru_rb, lru_iw, lru_ib, lru_lambda)

    zero_s = jnp.zeros((bc, SSD_GROUPS, SSD_HPG, SSD_HEADDIM, SSD_STATE), f32)
    yc_ssd, s_f, s_b = ssd_branch(zc, xbcc, dtc, *ssd_p, zero_s, zero_s)
    yl_ssd, _, _ = ssd_branch(zl, xbcl, dtl, *ssd_p, s_f, s_b)

    zero_h = jnp.zeros((bc, D_LRU), f32)
    hc, h_f, h_b = rglru_bidir(dwconv(lxc, lru_conv_w, lru_conv_b), *lru_p, zero_h, zero_h)
    yc_lru = rmsnorm(hc.astype(u_ctx.dtype) * jax.nn.gelu(lgc), lru_norm_w)
    cols = lxl.reshape(bsz, rows, GRID_W, D_LRU).transpose(0, 2, 1, 3).reshape(bsz * GRID_W, rows, D_LRU)
    xcol = dwconv(cols, lru_conv_w, lru_conv_b).reshape(bsz, t, D_LRU)
    hl, _, _ = rglru_bidir(xcol, *lru_p, h_f, h_b)
    hl = hl.reshape(bsz, GRID_W, rows, D_LRU).transpose(0, 2, 1, 3).reshape(bsz, t, D_LRU)
    yl_lru = rmsnorm(hl.astype(u_lat.dtype) * jax.nn.gelu(lgl), lru_norm_w)

    return (jnp.concatenate([yl_ssd, yl_lru], axis=-1), jnp.concatenate([yc_ssd, yc_lru], axis=-1))


def swiglu(h, w1, w3, w2):
    return (jax.nn.silu(h @ w1) * (h @ w3)) @ w2


def moe_ffn(h, router_w, w1, w3, w2):
    shp = h.shape
    tok = h.reshape(-1, shp[-1])
    logits = (tok @ router_w).astype(jnp.float32)
    top_v, top_i = lax.top_k(logits, TOP_K)
    top_p = jax.nn.softmax(top_v, axis=-1)
    combine = jnp.sum(top_p[..., None] * jax.nn.one_hot(top_i, N_EXPERTS, dtype=jnp.float32), axis=-2)
    combine = combine.astype(tok.dtype)
    out = jnp.zeros_like(tok)
    for e in range(N_EXPERTS):
        out = out + combine[:, e:e + 1] * swiglu(tok, w1[e], w3[e], w2[e])
    return out.reshape(shp)


def setup_inputs(seed: int = 0) -> dict:
    key = jax.random.key(seed)
    k = jax.random.split(key, 32)
    f32 = jnp.float32
    nrm = lambda i, shape, scale: scale * jax.random.normal(k[i], shape, f32)
    dt0 = jnp.exp(jax.random.uniform(k[11], (DEPTH, 2, SSD_HEADS), f32, math.log(1e-3), math.log(1e-1)))
    a0 = jax.random.uniform(k[21], (DEPTH, 2, D_LRU), f32, 0.9, 0.999)
    s0 = a0 ** (1.0 / RGLRU_C)
    return {
        "x": nrm(0, (BATCH, SEQ, D_MODEL), 1.0),
        "c": nrm(1, (BATCH, D_MODEL), 1.0),
        "ctx": nrm(2, (BATCH, CTX_LEN, D_MODEL), 1.0),
        "c_ctx": nrm(3, (D_MODEL,), 1.0),
        "mod_w": nrm(4, (DEPTH, D_MODEL, N_MOD * D_MODEL), 0.3 * D_MODEL ** -0.5),
        "mod_b": nrm(5, (DEPTH, N_MOD * D_MODEL), 0.02),
        "norm1_w": 1.0 + nrm(6, (DEPTH, D_MODEL), 0.1),
        "norm2_w": 1.0 + nrm(7, (DEPTH, D_MODEL), 0.1),
        "w_in": nrm(8, (DEPTH, D_MODEL, D_IN), D_MODEL ** -0.5),
        "ssd_conv_w": nrm(9, (DEPTH, CONV_K, D_XBC), 0.5),
        "ssd_conv_b": nrm(10, (DEPTH, D_XBC), 0.02),
        "ssd_dt_bias": dt0 + jnp.log(-jnp.expm1(-dt0)),
        "ssd_a_log": jnp.log(jax.random.uniform(k[12], (DEPTH, 2, SSD_HEADS), f32, 1.0, 16.0)),
        "ssd_d": 1.0 + nrm(13, (DEPTH, SSD_HEADS), 0.1),
        "ssd_norm_w": 1.0 + nrm(14, (DEPTH, D_SSD), 0.1),
        "lru_conv_w": nrm(15, (DEPTH, CONV_K, D_LRU), 0.5),
        "lru_conv_b": nrm(16, (DEPTH, D_LRU), 0.02),
        "lru_rw": nrm(17, (DEPTH, 2, LRU_BLOCKS, LRU_BW, LRU_BW), LRU_BW ** -0.5),
        "lru_rb": nrm(18, (DEPTH, 2, D_LRU), 0.1),
        "lru_iw": nrm(19, (DEPTH, 2, LRU_BLOCKS, LRU_BW, LRU_BW), LRU_BW ** -0.5),
        "lru_ib": nrm(20, (DEPTH, 2, D_LRU), 0.1),
        "lru_lambda": jnp.log(s0) - jnp.log1p(-s0),
        "lru_norm_w": 1.0 + nrm(22, (DEPTH, D_LRU), 0.1),
        "w_out": nrm(23, (DEPTH, D_MIX, D_MODEL), D_MIX ** -0.5),
        "ffn_w1": nrm(24, (N_DENSE, D_MODEL, D_FF), D_MODEL ** -0.5),
        "ffn_w3": nrm(25, (N_DENSE, D_MODEL, D_FF), D_MODEL ** -0.5),
        "ffn_w2": nrm(26, (N_DENSE, D_FF, D_MODEL), D_FF ** -0.5),
        "router_w": nrm(27, (N_MOE, D_MODEL, N_EXPERTS), D_MODEL ** -0.5),
        "moe_w1": nrm(28, (N_MOE, N_EXPERTS, D_MODEL, D_FF), D_MODEL ** -0.5),
        "moe_w3": nrm(29, (N_MOE, N_EXPERTS, D_MODEL, D_FF), D_MODEL ** -0.5),
        "moe_w2": nrm(30, (N_MOE, N_EXPERTS, D_FF, D_MODEL), D_FF ** -0.5),
        "final_norm_w": 1.0 + nrm(31, (D_MODEL,), 0.1),
    }


def reference(x, c, ctx, c_ctx, mod_w, mod_b, norm1_w, norm2_w, w_in, ssd_conv_w, ssd_conv_b, ssd_dt_bias,
              ssd_a_log, ssd_d, ssd_norm_w, lru_conv_w, lru_conv_b, lru_rw, lru_rb, lru_iw, lru_ib, lru_lambda,
              lru_norm_w, w_out, ffn_w1, ffn_w3, ffn_w2, router_w, moe_w1, moe_w3, moe_w2, final_norm_w):
    rows = x.shape[1] // GRID_W
    xc = ctx
    sc = jax.nn.silu(c)
    scc = jax.nn.silu(c_ctx)
    for l in range(DEPTH):
        last = l == DEPTH - 1
        mod = jnp.split((sc @ mod_w[l] + mod_b[l])[:, None, :], N_MOD, axis=-1)
        modc = jnp.split(scc @ mod_w[l] + mod_b[l], N_MOD, axis=-1)

        hx = modulate(rmsnorm(x, norm1_w[l]), mod[0], mod[1])
        hc = modulate(rmsnorm(xc, norm1_w[l]), modc[0], modc[1])
        ox, oc = hybrid_mixer(hx @ w_in[l], hc @ w_in[l], rows, ssd_conv_w[l], ssd_conv_b[l], ssd_dt_bias[l],
                              ssd_a_log[l], ssd_d[l], ssd_norm_w[l], lru_conv_w[l], lru_conv_b[l], lru_rw[l],
                              lru_rb[l], lru_iw[l], lru_ib[l], lru_lambda[l], lru_norm_w[l])
        x = x + mod[2] * (ox @ w_out[l])

        if l % 2 == 0:
            ffn = functools.partial(swiglu, w1=ffn_w1[l // 2], w3=ffn_w3[l // 2], w2=ffn_w2[l // 2])
        else:
            ffn = functools.partial(moe_ffn, router_w=router_w[l // 2], w1=moe_w1[l // 2],
                                    w3=moe_w3[l // 2], w2=moe_w2[l // 2])
        x = x + mod[5] * ffn(modulate(rmsnorm(x, norm2_w[l]), mod[3], mod[4]))

        if not last:
            xc = xc + modc[2] * (oc @ w_out[l])
            xc = xc + modc[5] * ffn(modulate(rmsnorm(xc, norm2_w[l]), modc[3], modc[4]))
    return rmsnorm(x, final_norm_w)
```

```python
import numpy as np
import concourse.bass as bass
import concourse.mybir as mybir
from concourse.bass_utils import run_bass_kernel_spmd

F32 = mybir.dt.float32
BF16 = mybir.dt.bfloat16
AF = mybir.ActivationFunctionType
ALU = mybir.AluOpType
AX = mybir.AxisListType

SAME_ENGINE_SYNC = True
N_DMA_SEMS = 12

NCORES = 8
NB = 2
T = 4352
TW = 256
NT = 17
DM = 1024
KC = 8
OFF_XBC = 1024
OFF_DT = 2560
OFF_LX = 2592
OFF_LG = 3616
DIN = 4640
DFF = 2816
FC = 22
NE = 8
EPS = 1e-6
NCH = 34

VOFF = {}
_o = 0
for _n, _w in [("n1w", 8), ("n2w", 8), ("modb", 48), ("scw", 48), ("scb", 12), ("snw", 8), ("lcw", 32), ("lcb", 8),
               ("lrb", 16), ("lib", 16), ("llam", 16), ("lnw", 8), ("sdv", 8), ("fnw", 8)]:
    VOFF[_n] = _o
    _o += _w
NV = _o


class K:
    def __init__(self, nc):
        self.nc = nc
        self.engs = {"pe": nc.tensor, "act": nc.scalar, "dve": nc.vector, "pool": nc.gpsimd, "sp": nc.sync}
        self.sem = {e: nc.semaphore("sem_" + e).__enter__() for e in self.engs}
        self.cnt = {e: 0 for e in self.engs}
        self.seen = {e: {} for e in self.engs}
        self.lw = {}
        self.rd = {}
        self.dsem = {}
        self.dcnt = {}
        self.dnext = {}
        for q in ("sp", "pool"):
            self.dsem[q] = [nc.semaphore("dsem_%s_%d" % (q, i)).__enter__() for i in range(N_DMA_SEMS)]
            self.dcnt[q] = [0] * N_DMA_SEMS
            self.dnext[q] = 0
        self.n_inst = 0

    def _semof(self, o):
        if isinstance(o, tuple):
            return self.dsem[o[1]][o[2]]
        return self.sem[o]

    def _wait(self, e, o, v):
        if o == e and (not SAME_ENGINE_SYNC or e == "pe"):
            return
        if self.seen[e].get(o, 0) >= v:
            return
        self.engs[e].wait_ge(self._semof(o), v)
        self.seen[e][o] = v

    def _deps(self, e, reads, writes):
        need = {}
        for k in reads:
            w = self.lw.get(k)
            if w is not None and need.get(w[0], 0) < w[1]:
                need[w[0]] = w[1]
        for k in writes:
            w = self.lw.get(k)
            if w is not None and need.get(w[0], 0) < w[1]:
                need[w[0]] = w[1]
            for o, v in self.rd.get(k, {}).items():
                if need.get(o, 0) < v:
                    need[o] = v
        for o, v in need.items():
            self._wait(e, o, v)

    def _record(self, who, v, reads, writes):
        for k in reads:
            self.rd.setdefault(k, {})[who] = v
        for k in writes:
            self.lw[k] = (who, v)
            self.rd[k] = {}

    def op(self, e, emit, reads=(), writes=()):
        self._deps(e, reads, writes)
        inst = emit(self.engs[e])
        self.cnt[e] += 1
        inst.then_inc(self.sem[e], 1)
        self._record(e, self.cnt[e], reads, writes)
        self.n_inst += 1
        return inst

    def dma(self, q, out, in_, reads=(), writes=(), **kw):
        j = self.dnext[q]
        self.dnext[q] = (j + 1) % N_DMA_SEMS
        who = ("dma", q, j)
        if self.dcnt[q][j] > 0:
            self._wait(q, who, self.dcnt[q][j])
        self._deps(q, reads, writes)
        inst = self.engs[q].dma_start(out=out, in_=in_, **kw)
        self.dcnt[q][j] += 16
        inst.then_inc(self.dsem[q][j], 16)
        self._record(who, self.dcnt[q][j], reads, writes)
        self.n_inst += 1
        return inst

    def barrier(self):
        for e in self.engs:
            for o in self.engs:
                if self.cnt[o] > 0:
                    self._wait(e, o, self.cnt[o])
            for q in self.dsem:
                for j in range(N_DMA_SEMS):
                    if self.dcnt[q][j] > 0:
                        self._wait(e, ("dma", q, j), self.dcnt[q][j])
        self.lw = {}
        self.rd = {}


class Scope:
    def __init__(self, nc):
        self.nc = nc
        self.guards = []

    _uid = [0]

    def sb(self, name, shape, dt=F32):
        Scope._uid[0] += 1
        g = self.nc.sbuf_tensor("%s_u%d" % (name, Scope._uid[0]), shape, dt)
        t = g.__enter__()
        self.guards.append(g)
        return t

    def close(self):
        for g in reversed(self.guards):
            g.__exit__(None, None, None)
        self.guards = []


def bc(ap, shape):
    return ap.broadcast_to(shape)


def build(nlayers=2, dbg=None, phases=("A", "B", "C", "D", "F"), lite=False):
    nc = bass.Bass("TRN2", target_bir_lowering=False)
    k = K(nc)
    dbg = dbg or {}

    def din(name, shape, dt=F32):
        return nc.dram_tensor(name, list(shape), dt, kind="ExternalInput").ap()

    def dint(name, shape, dt=F32):
        kind = "ExternalOutput" if name in dbg else "Internal"
        return nc.dram_tensor(name, list(shape), dt, kind=kind).ap()

    xin = din("xin", [NB, DM, T])
    cvec = din("cvec", [128, KC, 4])
    vecs = din("vecs", [2, 128, NV])
    consts = din("consts", [128, 4, 128])
    dtrow = din("dtrow", [2, 2, 32])
    mod_w = din("mod_w", [2, DM, 6 * DM])
    w_in = din("w_in", [2, DM, DIN])
    lru_rw = din("lru_rw", [2, 2, 16, 64, 64])
    lru_iw = din("lru_iw", [2, 2, 16, 64, 64])
    w_out = din("w_out", [2, 2 * DM, DM])
    ffn_w1 = din("ffn_w1", [1, DM, DFF])
    ffn_w3 = din("ffn_w3", [1, DM, DFF])
    ffn_w2 = din("ffn_w2", [1, DFF, DM])
    router_w = din("router_w", [1, DM, NE])
    if lite:
        moe_w1 = moe_w3 = moe_w2 = None
    else:
        moe_w1 = din("moe_w1", [1, NE, DM, DFF])
        moe_w3 = din("moe_w3", [1, NE, DM, DFF])
        moe_w2 = din("moe_w2", [1, NE, DFF, DM])
    out = nc.dram_tensor("out", [NB, DM, 4096], F32, kind="ExternalOutput").ap()

    XR = dint("XR", [NB, DM, T])
    HTD = dint("HTD", [NB, DM, T], BF16)
    YD = dint("YD", [NB, 2 * DM, T], BF16)
    RSD = dint("RSD", [NB, 2, T])
    H2D = dint("H2D", [DM, NB * T], BF16)
    CWD = dint("CWD", [NE, NB * T])
    GROWD = dint("GROWD", [NCH, 32 * 128])
    HBD = dint("HBD", [NCH, 128, DM], BF16)
    DBGB = dint("DBGB", [8, 128, T]) if "DBGB" in dbg else None

    P = Scope(nc)
    CONST = P.sb("CONST", [128, 4, 128])
    ONES = CONST[:, 0, :]
    TRIF = CONST[:, 1, :]
    TRIB = CONST[:, 2, :]
    IDENTF = CONST[:, 3, :]
    IDENTB = P.sb("IDENTB", [128, 128], BF16)
    CV = P.sb("CV", [128, KC, 4])
    SC = P.sb("SC", [128, KC, 4])
    VEC = P.sb("VEC", [128, NV])
    MODV = P.sb("MODV", [128, 48, 4])
    MODP = P.sb("MODP", [128, 6, KC, 4])
    SCL = P.sb("SCL", [128, 16])
    SCL2 = P.sb("SCL2", [128, 16])
    STG = [P.sb("STG%d" % i, [128, 1024]) for i in range(2)]
    PS = [nc.psum_tensor("PS%d" % i, [128, 512], F32).__enter__() for i in range(8)]
    stg_i = [0]

    def V(name, i=0, n=1):
        o = VOFF[name] + i
        return VEC[:, o:o + n]

    k.dma("sp", CONST[:], consts[:, :, :], writes=["CONST"])
    k.op("pool", lambda e: e.tensor_copy(out=IDENTB[:], in_=IDENTF), reads=["CONST"], writes=["IDENTB"])
    k.dma("sp", CV[:], cvec[:, :, :], writes=["CV"])
    k.op("act", lambda e: e.activation(out=SC[:], in_=CV[:], func=AF.Silu), reads=["CV"], writes=["SC"])

    stg_extra = []

    def load_cast(dst, src2d, kc, ncols, key, scale_vec=None, q="sp"):
        pc = min(ncols, 1024)
        while ncols % pc != 0:
            pc -= 1
        srcv = src2d.rearrange("(k p) n -> p k n", p=128)
        for kk in range(kc):
            for c0 in range(0, ncols, pc):
                bufs = STG + stg_extra
                si = stg_i[0] % len(bufs)
                stg_i[0] += 1
                sv = bufs[si][:, 0:pc]
                k.dma(q, sv, srcv[:, kk, c0:c0 + pc], writes=[("STG", si)])
                eng = "dve" if (stg_i[0] % 4) < 2 else "act"
                sc = None if scale_vec is None else scale_vec(kk)
                rk = [("STG", si)] + ([] if sc is None else ["VEC"])
                if eng == "dve":
                    if sc is None:
                        k.op("dve", lambda e, sv=sv, c0=c0, kk=kk: e.tensor_copy(out=dst[:, kk, c0:c0 + pc], in_=sv),
                             reads=rk, writes=[key])
                    else:
                        k.op("dve", lambda e, sv=sv, c0=c0, kk=kk, sc=sc: e.tensor_scalar(
                            out=dst[:, kk, c0:c0 + pc], in0=sv, scalar1=sc, scalar2=None, op0=ALU.mult),
                            reads=rk, writes=[key])
                else:
                    if sc is None:
                        k.op("act", lambda e, sv=sv, c0=c0, kk=kk: e.activation(out=dst[:, kk, c0:c0 + pc], in_=sv,
                                                                                 func=AF.Identity),
                             reads=rk, writes=[key])
                    else:
                        k.op("act", lambda e, sv=sv, c0=c0, kk=kk, sc=sc: e.activation(
                            out=dst[:, kk, c0:c0 + pc], in_=sv, func=AF.Identity, scale=sc), reads=rk, writes=[key])

    def rstd_from_ms(eng_out, src, key_r, key_w):
        k.op("act", lambda e: e.activation(out=eng_out, in_=src, func=AF.Sqrt, scale=1.0 / DM, bias=EPSV[:, 0:1]),
             reads=key_r + ["EPSV"], writes=key_w)
        k.op("dve", lambda e: e.reciprocal(out=eng_out, in_=eng_out), reads=key_w, writes=key_w)

    EPSV = P.sb("EPSV", [128, 2])
    k.op("dve", lambda e: e.memset(EPSV[:, 0:1], EPS), writes=["EPSV"])
    k.op("dve", lambda e: e.memset(EPSV[:, 1:2], 1.0), reads=["EPSV"], writes=["EPSV"])

    def layer_setup(l):
        k.dma("sp", VEC[:], vecs[l, :, :], writes=["VEC"])
        for pc in range(48):
            si = stg_i[0] % 2
            stg_i[0] += 1
            sv = STG[si][:, :].rearrange("p (k n) -> p k n", k=KC)
            k.dma("sp", sv, mod_w[l].rearrange("(k p) n -> p k n", p=128)[:, :, pc * 128:(pc + 1) * 128],
                  writes=[("STG", si)])
            for h in range(1):
                oc = pc + h
                for kk in range(KC):
                    k.op("pe", lambda e, kk=kk, h=h, sv=sv: e.matmul(
                        PS[0][:, h * 4:h * 4 + 4], lhsT=sv[:, kk, h * 128:(h + 1) * 128], rhs=SC[:, kk, :],
                        start=(kk == 0), stop=(kk == KC - 1)), reads=[("STG", si), "SC"], writes=[("PS", 0)])
                k.op("dve", lambda e, oc=oc, h=h: e.tensor_scalar(
                    out=MODV[:, oc, :], in0=PS[0][:, h * 4:h * 4 + 4], scalar1=V("modb", oc), scalar2=None,
                    op0=ALU.add), reads=[("PS", 0), "VEC"], writes=["MODV"])
        for s, (nwn, ish, isc, igt) in enumerate([("n1w", 0, 1, 2), ("n2w", 3, 4, 5)]):
            k.op("dve", lambda e, s=s, isc=isc: e.tensor_scalar(
                out=MODP[:, 3 * s, :, :], in0=MODV[:, isc * 8:isc * 8 + 8, :], scalar1=1.0, scalar2=None,
                op0=ALU.add), reads=["MODV"], writes=["MODP"])
            k.op("dve", lambda e, s=s, nwn=nwn: e.tensor_tensor(
                out=MODP[:, 3 * s, :, :], in0=MODP[:, 3 * s, :, :],
                in1=bc(V(nwn, 0, 8).unsqueeze(2), [128, 8, 4]), op=ALU.mult), reads=["MODP", "VEC"], writes=["MODP"])
            k.op("dve", lambda e, s=s, ish=ish: e.tensor_copy(
                out=MODP[:, 3 * s + 1, :, :], in_=MODV[:, ish * 8:ish * 8 + 8, :]), reads=["MODV"], writes=["MODP"])
            k.op("dve", lambda e, s=s, igt=igt: e.tensor_copy(
                out=MODP[:, 3 * s + 2, :, :], in_=MODV[:, igt * 8:igt * 8 + 8, :]), reads=["MODV"], writes=["MODP"])
        k.op("act", lambda e: e.activation(out=SCL[:], in_=V("llam", 0, 16), func=AF.Exp, scale=-1.0),
             reads=["VEC"], writes=["SCL"])
        k.op("act", lambda e: e.activation(out=SCL[:], in_=SCL[:], func=AF.Ln, bias=EPSV[:, 1:2]),
             reads=["SCL", "EPSV"], writes=["SCL"])
        k.op("dve", lambda e: e.tensor_scalar(out=SCL2[:], in0=SCL[:], scalar1=-16.0, scalar2=None, op0=ALU.mult),
             reads=["SCL"], writes=["SCL2"])
        k.op("dve", lambda e: e.tensor_scalar(out=SCL[:], in0=SCL[:], scalar1=-8.0, scalar2=None, op0=ALU.mult),
             reads=["SCL", "SCL2"], writes=["SCL"])

    def xsrc_of(l):
        return xin if l == 0 else XR

    def xtile(src, b, t):
        return src[b].rearrange("(c p) t -> p c t", p=128)[:, :, t * TW:(t + 1) * TW]

    def norm_s1(S, par, psb):
        XT, SQ, RS, XN = S["XT"][par], S["SQ"][par], S["RS"][par], S["XN"][par]
        k.op("act", lambda e: e.activation(out=SQ[:], in_=XT[:], func=AF.Square), reads=[("XT", par)],
             writes=[("SQ", par)])
        for c in range(KC):
            k.op("pe", lambda e, c=c: e.matmul(PS[psb][:, 0:TW], lhsT=ONES, rhs=SQ[:, c, :], start=(c == 0),
                                               stop=(c == KC - 1)), reads=[("SQ", par), "CONST"], writes=[("PS", psb)])
        rstd_from_ms(RS[:], PS[psb][:, 0:TW], [("PS", psb)], [("RS", par)])
        k.op("dve", lambda e: e.tensor_tensor(out=XN[:], in0=XT[:], in1=bc(RS[:].unsqueeze(1), [128, KC, TW]),
                                              op=ALU.mult), reads=[("XT", par), ("RS", par), ("XN", par)],
             writes=[("XN", par)])

    def norm_s2(S, par, mslot, col, dst_fn, extra_reads=()):
        XN = S["XN"][par]
        for c in range(KC):
            dst, wkeys = dst_fn(c)
            k.op("act", lambda e, c=c, dst=dst: e.activation(
                out=dst, in_=XN[:, c, :], func=AF.Identity, scale=MODP[:, mslot, c, col:col + 1],
                bias=MODP[:, mslot + 1, c, col:col + 1]), reads=[("XN", par), "MODP"] + list(extra_reads),
                writes=wkeys)

    def norm_pipeline(S, n, load, stage2):
        load(0)
        if n > 1:
            load(1)
        for i in range(n):
            norm_s1(S, i % 2, i % 2)
            if i + 2 < n:
                load(i + 2)
            if i >= 1:
                stage2(i - 1)
        stage2(n - 1)

    def phase_A(l, b, HT):
        S = Scope(nc)
        st = {n: [S.sb("%s%d" % (n, i), [128, KC, TW]) for i in range(2)] for n in ("XT", "SQ", "XN")}
        st["RS"] = [S.sb("RS%d" % i, [128, TW]) for i in range(2)]
        src = xsrc_of(l)
        def load(t):
            k.dma("sp", st["XT"][t % 2][:], xtile(src, b, t), reads=[("XR", b, t)], writes=[("XT", t % 2)])

        def stage2(t):
            col = 2 if t == 0 else b
            norm_s2(st, t % 2, 0, col, lambda c, t=t: (HT[:, c, t * TW:(t + 1) * TW], [("HT", t)]))
            k.dma("sp", xtile(HTD, b, t), HT[:, :, t * TW:(t + 1) * TW], reads=[("HT", t)], writes=[("HTD", t)])

        norm_pipeline(st, NT, load, stage2)
        k.barrier()
        S.close()

    def phase_B(l, b, HT):
        S = Scope(nc)
        XL = S.sb("XL", [128, T])
        XC = S.sb("XC", [128, T])
        RR = S.sb("RR", [128, T])
        II = S.sb("II", [128, T])
        HF = S.sb("HF", [128, T])
        WLX = S.sb("WLX", [128, KC, 128], BF16)
        WLG = S.sb("WLG", [128, KC, 128], BF16)
        BDS = S.sb("BDS", [128, 4, 128])
        GE = [S.sb("GE%d" % i, [128, TW]) for i in range(2)]
        YB = [S.sb("YBl%d" % i, [128, TW], BF16) for i in range(2)]
        k.op("dve", lambda e: e.memset(BDS[:], 0.0), writes=["BDS"])
        pieces = [(0, 256)] + [(256 + i * 512, 256 + (i + 1) * 512) for i in range(8)]
        NP = len(pieces)
        xl_keys = ["XL"] + [("XLp", p) for p in range(NP)]
        hf_keys = ["HF"] + [("HFp", p) for p in range(NP)]
        rr_keys = [("RR", p) for p in range(NP)]
        ii_keys = [("II", p) for p in range(NP)]

        def scan_view(buf, t):
            r0 = (t - 1) * 4
            return buf[:, 256:].rearrange("p (c r) -> p r c", r=64)[:, r0:r0 + 4, :]

        def seg(buf, sgi, lo, hi):
            if sgi == 0:
                return buf[:, 0:256].rearrange("p (a r) -> p a r", a=1)[:, :, lo:hi]
            return buf[:, 256:].rearrange("p (c r) -> p c r", r=64)[:, :, lo:hi]

        def load_wlx(j):
            load_cast(WLX, w_in[l][:, OFF_LX + j * 128:OFF_LX + (j + 1) * 128], KC, 128, "WLX")

        def load_wlg(j):
            load_cast(WLG, w_in[l][:, OFF_LG + j * 128:OFF_LG + (j + 1) * 128], KC, 128, "WLG")

        def load_bd(j):
            for q, (wt, d) in enumerate([(lru_rw, 0), (lru_iw, 0), (lru_rw, 1), (lru_iw, 1)]):
                for h in range(2):
                    k.dma("sp", BDS[h * 64:(h + 1) * 64, q, h * 64:(h + 1) * 64], wt[l, d, 2 * j + h, :, :],
                          reads=["BDS"], writes=["BDS"])

        load_wlx(0)
        load_bd(0)
        for j in range(KC):
            load_wlg(j)
            for t in range(NT):
                pb = t % 2
                for kk in range(KC):
                    k.op("pe", lambda e, kk=kk, t=t, pb=pb: e.matmul(
                        PS[pb][:, 0:TW], lhsT=WLX[:, kk, :], rhs=HT[:, kk, t * TW:(t + 1) * TW], start=(kk == 0),
                        stop=(kk == KC - 1)), reads=["WLX", ("HT", t)], writes=[("PS", pb)])
                if t == 0:
                    k.op("dve", lambda e, pb=pb: e.tensor_copy(out=XL[:, 0:256], in_=PS[pb][:, 0:TW]),
                         reads=[("PS", pb)] + xl_keys, writes=xl_keys)
                else:
                    k.op("act" if t % 2 == 0 else "dve",
                         (lambda e, pb=pb, t=t: e.activation(
                             out=scan_view(XL, t), in_=PS[pb][:, 0:TW].rearrange("p (r c) -> p r c", c=64),
                             func=AF.Identity)) if t % 2 == 0 else
                         (lambda e, pb=pb, t=t: e.tensor_copy(
                             out=scan_view(XL, t), in_=PS[pb][:, 0:TW].rearrange("p (r c) -> p r c", c=64))),
                         reads=[("PS", pb)] + xl_keys, writes=xl_keys)
            if j + 1 < KC:
                load_wlx(j + 1)
            k.op("dve", lambda e, j=j: e.tensor_scalar(out=XC[:], in0=XL[:], scalar1=V("lcw", 2 * 8 + j),
                                                        scalar2=V("lcb", j), op0=ALU.mult, op1=ALU.add),
                 reads=xl_keys + ["VEC", "XC"], writes=["XC"])
            for tap, sh in [(0, -2), (1, -1), (3, 1)]:
                for sgi in range(2):
                    L = 256 if sgi == 0 else 64
                    if sh < 0:
                        o_lo, o_hi, i_lo, i_hi = -sh, L, 0, L + sh
                    else:
                        o_lo, o_hi, i_lo, i_hi = 0, L - sh, sh, L
                    ov = seg(XC, sgi, o_lo, o_hi)
                    iv = seg(XL, sgi, i_lo, i_hi)
                    k.op("dve", lambda e, ov=ov, iv=iv, tap=tap, j=j: e.scalar_tensor_tensor(
                        out=ov, in0=iv, scalar=V("lcw", tap * 8 + j), in1=ov, op0=ALU.mult, op1=ALU.add),
                        reads=xl_keys + ["XC", "VEC"], writes=["XC"])
            for d in range(2):
                order = list(range(NP)) if d == 0 else [0] + list(range(NP - 1, 0, -1))
                def stage_x(oi, p, d=d):
                    lo, hi = pieces[p]
                    w = hi - lo
                    sl = slice(lo, hi)
                    pr, pi = 2 + 2 * (oi % 2), 3 + 2 * (oi % 2)
                    k.op("pe", lambda e, pr=pr, sl=sl, d=d, w=w: e.matmul(
                        PS[pr][:, 0:w], lhsT=BDS[:, 2 * d, :], rhs=XC[:, sl], start=True, stop=True),
                        reads=["BDS", "XC"], writes=[("PS", pr)])
                    k.op("pe", lambda e, pi=pi, sl=sl, d=d, w=w: e.matmul(
                        PS[pi][:, 0:w], lhsT=BDS[:, 2 * d + 1, :], rhs=XC[:, sl], start=True, stop=True),
                        reads=["BDS", "XC"], writes=[("PS", pi)])
                    k.op("act", lambda e, pr=pr, sl=sl, d=d, j=j, w=w: e.activation(
                        out=RR[:, sl], in_=PS[pr][:, 0:w], func=AF.Sigmoid, bias=V("lrb", d * 8 + j)),
                        reads=[("PS", pr), "VEC", ("RR", p)], writes=[("RR", p)])
                    k.op("act", lambda e, pi=pi, sl=sl, d=d, j=j, w=w: e.activation(
                        out=II[:, sl], in_=PS[pi][:, 0:w], func=AF.Sigmoid, bias=V("lib", d * 8 + j)),
                        reads=[("PS", pi), "VEC", ("II", p)], writes=[("II", p)])
                    k.op("act", lambda e, d=d, j=j, sl=sl: e.activation(
                        out=XL[:, sl], in_=RR[:, sl], func=AF.Exp, scale=SCL[:, d * 8 + j:d * 8 + j + 1]),
                        reads=[("RR", p), "SCL", ("XLp", p), "XL"], writes=[("XLp", p)])
                    k.op("dve", lambda e, sl=sl: e.tensor_scalar(out=XL[:, sl], in0=XL[:, sl], scalar1=1.0,
                                                                  scalar2=None, op0=ALU.min),
                         reads=[("XLp", p)], writes=[("XLp", p)])
                    k.op("dve", lambda e, sl=sl: e.tensor_tensor(out=RR[:, sl], in0=XL[:, sl], in1=XL[:, sl],
                                                                  op=ALU.mult),
                         reads=[("RR", p), ("XLp", p)], writes=[("RR", p)])

                def stage_y(oi, p, d=d):
                    lo, hi = pieces[p]
                    w = hi - lo
                    sl = slice(lo, hi)
                    k.op("act", lambda e, sl=sl: e.activation(out=RR[:, sl], in_=RR[:, sl], func=AF.Sqrt,
                                                              scale=-1.0, bias=EPSV[:, 1:2]),
                         reads=[("RR", p), "EPSV"], writes=[("RR", p)])
                    k.op("dve", lambda e, sl=sl: e.tensor_tensor(out=II[:, sl], in0=II[:, sl], in1=RR[:, sl],
                                                                  op=ALU.mult),
                         reads=[("RR", p), ("II", p)], writes=[("II", p)])
                    k.op("dve", lambda e, sl=sl: e.tensor_tensor(out=II[:, sl], in0=II[:, sl], in1=XC[:, sl],
                                                                  op=ALU.mult),
                         reads=[("II", p), "XC"], writes=[("II", p)])
                    if d == 0:
                        init = 0.0 if p == 0 else HF[:, lo - 1:lo]
                        prev = [] if p == 0 else [("HFp", p - 1)]
                        k.op("dve", lambda e, sl=sl, init=init: e.tensor_tensor_scan(
                            out=HF[:, sl], data0=XL[:, sl], data1=II[:, sl], initial=init, op0=ALU.mult,
                            op1=ALU.add), reads=[("XLp", p), ("II", p), ("HFp", p), "HF"] + prev,
                            writes=[("HFp", p)])
                    else:
                        if p == 0:
                            init, prev = 0.0, []
                        elif p == NP - 1:
                            init, prev = RR[:, 0:1], [("RR", 0)]
                        else:
                            init, prev = RR[:, hi:hi + 1], [("RR", p + 1)]
                        k.op("dve", lambda e, sl=sl, init=init: e.tensor_tensor_scan(
                            out=RR[:, sl][:, ::-1], data0=XL[:, sl][:, ::-1], data1=II[:, sl][:, ::-1],
                            initial=init, op0=ALU.mult, op1=ALU.add),
                            reads=[("XLp", p), ("II", p), ("RR", p)] + prev, writes=[("RR", p)])
                        k.op("dve", lambda e, sl=sl: e.tensor_tensor(out=HF[:, sl], in0=HF[:, sl], in1=RR[:, sl],
                                                                      op=ALU.add),
                             reads=[("HFp", p), ("RR", p)], writes=[("HFp", p)])

                stage_x(0, order[0])
                for oi, p in enumerate(order):
                    if oi + 1 < NP:
                        stage_x(oi + 1, order[oi + 1])
                    stage_y(oi, p)
            if j + 1 < KC:
                load_bd(j + 1)
            for t in range(NT):
                pb = t % 2
                for kk in range(KC):
                    k.op("pe", lambda e, kk=kk, t=t, pb=pb: e.matmul(
                        PS[pb][:, 0:TW], lhsT=WLG[:, kk, :], rhs=HT[:, kk, t * TW:(t + 1) * TW], start=(kk == 0),
                        stop=(kk == KC - 1)), reads=["WLG", ("HT", t)], writes=[("PS", pb)])
                k.op("act", lambda e, pb=pb: e.activation(out=GE[pb][:], in_=PS[pb][:, 0:TW], func=AF.Gelu),
                     reads=[("PS", pb), ("GE", pb)], writes=[("GE", pb)])
                if t == 0:
                    k.op("dve", lambda e, pb=pb: e.tensor_tensor(out=YB[pb][:], in0=GE[pb][:], in1=HF[:, 0:256],
                                                                  op=ALU.mult),
                         reads=[("GE", pb), ("YB", pb)] + hf_keys, writes=[("YB", pb)])
                else:
                    k.op("dve", lambda e, pb=pb, t=t: e.tensor_tensor(
                        out=YB[pb][:].rearrange("p (r c) -> p r c", c=64),
                        in0=GE[pb][:].rearrange("p (r c) -> p r c", c=64), in1=scan_view(HF, t), op=ALU.mult),
                        reads=[("GE", pb), ("YB", pb)] + hf_keys, writes=[("YB", pb)])
                k.dma("sp", YD[b, DM + j * 128:DM + (j + 1) * 128, t * TW:(t + 1) * TW], YB[pb][:],
                      reads=[("YB", pb)], writes=[("YD", t)])
        k.barrier()
        S.close()

    def phase_C(l, b):
        S = Scope(nc)
        WX = S.sb("WX", [128, KC, 1536], BF16)
        WZ = S.sb("WZ", [128, KC, 1024], BF16)
        WDT = S.sb("WDT", [128, KC, 32], BF16)
        DTB = S.sb("DTB", [128, 32])
        ANEG = S.sb("ANEG", [128, 32])
        DT = S.sb("DT", [128, NCH, 32])
        GT = S.sb("GT", [128, NCH, 32])
        EGT = S.sb("EGT", [128, NCH, 32])
        WTS = S.sb("WTS", [128, NCH, 32])
        ATS = [S.sb("ATS%d" % i, [128, 32]) for i in range(2)]
        TMPA = [S.sb("TMPA%d" % i, [128, 32]) for i in range(2)]
        TMPB = [S.sb("TMPB%d" % i, [128, 32]) for i in range(2)]
        GROWS = [S.sb("GROWS%d" % i, [32, 128]) for i in range(2)]
        HTT = [S.sb("HTT%d" % i, [128, KC, TW + 3], BF16) for i in range(2)]
        RAW = [S.sb("RAW%d" % i, [128, TW + 3]) for i in range(2)]
        ACC = [S.sb("ACC%d" % i, [128, TW]) for i in range(2)]
        XBC = [S.sb("XBC%d" % i, [128, 12, TW], BF16) for i in range(2)]
        SZ = [S.sb("SZ%d" % i, [128, KC, TW], BF16) for i in range(2)]
        TOK = [S.sb("TOK%d" % i, [128, 1280], BF16) for i in range(2)]
        XF = [S.sb("XF%d" % i, [128, 16, 64], BF16) for i in range(2)]
        XB = [S.sb("XB%d" % i, [128, 16, 64], BF16) for i in range(2)]
        XD = S.sb("XD", [128, 16, 64], BF16)
        ACB = S.sb("ACB", [128, 32, 128])
        EB = S.sb("EB", [128, 32, 128])
        WW = [S.sb("WW%d" % i, [128, 32, 128], BF16) for i in range(2)]
        CS = [S.sb("CS%d" % i, [128, 32, 128], BF16) for i in range(2)]
        CBM1 = S.sb("CBM", [128, 4, 128])
        CBM = [CBM1, CBM1]
        YFs = S.sb("YFs", [128, KC, 128])
        YBs = S.sb("YBs", [128, KC, 128], BF16)
        HS = [S.sb("HS%d" % i, [128, 16, 64]) for i in range(2)]
        HSBF = [S.sb("HSBF%d" % i, [128, 16, 64], BF16) for i in range(2)]
        HSBB = [S.sb("HSBB%d" % i, [128, 16, 64], BF16) for i in range(2)]

        load_cast(WX, w_in[l][:, OFF_XBC:OFF_XBC + 1536], KC, 1536, "WX")
        load_cast(WZ, w_in[l][:, 0:1024], KC, 1024, "WZ")
        load_cast(WDT, w_in[l][:, OFF_DT:OFF_DT + 32], KC, 32, "WDT")
        k.dma("sp", DTB[:], dtrow[l, 0:1, :].partition_broadcast(128), writes=["DTB"])
        k.dma("sp", ANEG[:], dtrow[l, 1:2, :].partition_broadcast(128), writes=["ANEG"])
        k.op("act", lambda e: e.activation(out=ANEG[:], in_=ANEG[:], func=AF.Exp), reads=["ANEG"], writes=["ANEG"])
        k.op("dve", lambda e: e.tensor_scalar(out=ANEG[:], in0=ANEG[:], scalar1=-1.0, scalar2=None, op0=ALU.mult),
             reads=["ANEG"], writes=["ANEG"])

        def tok_range(t):
            lo, hi = (0, 256) if t == 0 else (256, T)
            return t * TW, lo, hi

        def load_ht(t, par):
            t0, lo, hi = tok_range(t)
            a, bnd = max(t0 - 2, lo), min(t0 + TW + 1, hi)
            if a > t0 - 2:
                k.op("pool", lambda e: e.memset(HTT[par][:, :, 0:2], 0.0), reads=[("HTT", par)],
                     writes=[("HTT", par)])
            if bnd < t0 + TW + 1:
                k.op("pool", lambda e: e.memset(HTT[par][:, :, TW + 2:TW + 3], 0.0), reads=[("HTT", par)],
                     writes=[("HTT", par)])
            k.dma("sp", HTT[par][:, :, a - (t0 - 2):bnd - (t0 - 2)],
                  HTD[b].rearrange("(c p) t -> p c t", p=128)[:, :, a:bnd], reads=[("HTT", par)],
                  writes=[("HTT", par)])

        def pp1_p(ci):
            t, hf = ci // 2, ci % 2
            par, q = t % 2, ci % 2
            if hf == 0 and t + 1 < NT:
                load_ht(t + 1, (t + 1) % 2)
            c0 = 2 + hf * 128
            pb = 3 * q
            tm, at = TMPA[q], ATS[q]
            for kk in range(KC):
                k.op("pe", lambda e, kk=kk: e.matmul(
                    PS[pb][:, 0:32], lhsT=HTT[par][:, kk, c0:c0 + 128], rhs=WDT[:, kk, :], start=(kk == 0),
                    stop=(kk == KC - 1)), reads=[("HTT", par), "WDT"], writes=[("PS", pb)])
            k.op("dve", lambda e: e.tensor_tensor(out=tm[:], in0=PS[pb][:, 0:32], in1=DTB[:], op=ALU.add),
                 reads=[("PS", pb), "DTB", ("TMPA", q)], writes=[("TMPA", q)])
            k.op("act", lambda e: e.activation(out=tm[:], in_=tm[:], func=AF.Exp), reads=[("TMPA", q)],
                 writes=[("TMPA", q)])
            k.op("act", lambda e: e.activation(out=DT[:, ci, :], in_=tm[:], func=AF.Ln, bias=EPSV[:, 1:2]),
                 reads=[("TMPA", q), "EPSV"], writes=[("DT", ci)])
            k.op("dve", lambda e: e.tensor_tensor(out=at[:], in0=DT[:, ci, :], in1=ANEG[:], op=ALU.mult),
                 reads=[("DT", ci), "ANEG", ("ATS", q)], writes=[("ATS", q)])

        def pp1_q(ci):
            q = ci % 2
            p1, p2 = 1 + 3 * q, 2 + 3 * q
            at, tb = ATS[q], TMPB[q]
            rk = [("ATS", q), "CONST"]
            k.op("pe", lambda e: e.matmul(PS[p1][:, 0:16], lhsT=TRIF, rhs=at[:, 0:16], start=True, stop=True),
                 reads=rk, writes=[("PS", p1)])
            k.op("pe", lambda e: e.matmul(PS[p1][:, 16:32], lhsT=TRIB, rhs=at[:, 16:32], start=True, stop=True),
                 reads=rk, writes=[("PS", p1)])
            k.op("pe", lambda e: e.matmul(PS[p1][:, 32:64], lhsT=ONES, rhs=at[:, :], start=True, stop=True),
                 reads=rk, writes=[("PS", p1)])
            k.op("pe", lambda e: e.matmul(PS[p2][0:32, 0:128], lhsT=at[:, 0:32], rhs=TRIF, start=True, stop=True),
                 reads=rk, writes=[("PS", p2)])
            k.op("pe", lambda e: e.matmul(PS[p2][0:32, 128:256], lhsT=at[:, 0:32], rhs=TRIB, start=True, stop=True),
                 reads=rk, writes=[("PS", p2)])
            k.op("dve", lambda e: e.tensor_copy(out=GT[:, ci, :], in_=PS[p1][:, 0:32]), reads=[("PS", p1)],
                 writes=[("GT", ci)])
            k.op("act", lambda e: e.activation(out=EGT[:, ci, :], in_=PS[p1][:, 32:64], func=AF.Exp),
                 reads=[("PS", p1)], writes=[("EGT", ci)])
            k.op("dve", lambda e: e.tensor_tensor(out=tb[:], in0=PS[p1][:, 32:64], in1=GT[:, ci, :],
                                                  op=ALU.subtract), reads=[("PS", p1), ("GT", ci), ("TMPB", q)],
                 writes=[("TMPB", q)])
            k.op("act", lambda e: e.activation(out=tb[:], in_=tb[:], func=AF.Exp), reads=[("TMPB", q)],
                 writes=[("TMPB", q)])
            k.op("dve", lambda e: e.tensor_tensor(out=WTS[:, ci, :], in0=tb[:], in1=DT[:, ci, :], op=ALU.mult),
                 reads=[("TMPB", q), ("DT", ci)], writes=[("WTS", ci)])
            k.op("dve", lambda e: e.tensor_scalar(out=GROWS[q][:], in0=PS[p2][0:32, 0:128],
                                                  scalar1=CONST[0:32, 1, 15:16], scalar2=None, op0=ALU.mult),
                 reads=[("PS", p2), ("GROWS", q), "CONST"], writes=[("GROWS", q)])
            k.op("dve", lambda e: e.scalar_tensor_tensor(
                out=GROWS[q][:], in0=PS[p2][0:32, 128:256], scalar=CONST[0:32, 2, 16:17], in1=GROWS[q][:],
                op0=ALU.mult, op1=ALU.add), reads=[("PS", p2), ("GROWS", q), "CONST"], writes=[("GROWS", q)])
            k.dma("sp", GROWD[ci].rearrange("(h l) -> h l", h=32), GROWS[q][:], reads=[("GROWS", q)],
                  writes=["GROWD"])

        load_ht(0, 0)
        pp1_p(0)
        for ci in range(NCH):
            if ci + 1 < NCH:
                pp1_p(ci + 1)
            pp1_q(ci)

        ccn = [0]

        def front(t, par, full):
            load_ht(t, par)
            xb = XBC[par]
            ncc = 12 if full else 10
            pend = []

            def silu_cc(cc, ai):
                k.op("act", lambda e: e.activation(out=xb[:, cc, :], in_=ACC[ai][:], func=AF.Silu),
                     reads=[("ACC", ai)], writes=[("XBC", par, cc)])

            for cc in range(ncc):
                pb = 4 + cc % 2
                ri = ccn[0] % 2
                ai = ccn[0] % 2
                ccn[0] += 1
                for kk in range(KC):
                    k.op("pe", lambda e, kk=kk, cc=cc, pb=pb: e.matmul(
                        PS[pb][:, 0:TW + 3], lhsT=WX[:, kk, cc * 128:(cc + 1) * 128], rhs=HTT[par][:, kk, :],
                        start=(kk == 0), stop=(kk == KC - 1)), reads=["WX", ("HTT", par)], writes=[("PS", pb)])
                k.op("act", lambda e, pb=pb, ri=ri: e.activation(out=RAW[ri][:], in_=PS[pb][:, 0:TW + 3],
                                                                  func=AF.Identity), reads=[("PS", pb), ("RAW", ri)],
                     writes=[("RAW", ri)])
                if pend:
                    silu_cc(*pend.pop(0))
                k.op("dve", lambda e, cc=cc, ri=ri, ai=ai: e.tensor_scalar(
                    out=ACC[ai][:], in0=RAW[ri][:, 2:2 + TW], scalar1=V("scw", 2 * 12 + cc), scalar2=V("scb", cc),
                    op0=ALU.mult, op1=ALU.add), reads=[("RAW", ri), "VEC", ("ACC", ai)], writes=[("ACC", ai)])
                for tap, off in [(0, 0), (1, 1), (3, 3)]:
                    k.op("dve", lambda e, cc=cc, tap=tap, off=off, ri=ri, ai=ai: e.scalar_tensor_tensor(
                        out=ACC[ai][:], in0=RAW[ri][:, off:off + TW], scalar=V("scw", tap * 12 + cc),
                        in1=ACC[ai][:], op0=ALU.mult, op1=ALU.add), reads=[("RAW", ri), ("ACC", ai), "VEC"],
                        writes=[("ACC", ai)])
                pend.append((cc, ai))
            while pend:
                silu_cc(*pend.pop(0))
            if full:
                for cc in range(KC):
                    pb = 4 + cc % 2
                    for kk in range(KC):
                        k.op("pe", lambda e, kk=kk, cc=cc, pb=pb: e.matmul(
                            PS[pb][:, 0:TW], lhsT=WZ[:, kk, cc * 128:(cc + 1) * 128], rhs=HTT[par][:, kk, 2:2 + TW],
                            start=(kk == 0), stop=(kk == KC - 1)), reads=["WZ", ("HTT", par)], writes=[("PS", pb)])
                    k.op("act", lambda e, cc=cc, pb=pb: e.activation(out=SZ[par][:, cc, :], in_=PS[pb][:, 0:TW],
                                                                      func=AF.Silu), reads=[("PS", pb)],
                         writes=[("SZ", par, cc)])

        def to_tok(par, hf, tp):
            for rnd in range(2):
                for i in range(5):
                    cc = rnd * 5 + i
                    k.op("pe", lambda e, cc=cc, i=i: e.transpose(
                        PS[6][:, i * 64:(i + 1) * 64].bitcast(BF16), XBC[par][:, cc, hf * 128:(hf + 1) * 128],
                        IDENTB[:]), reads=[("XBC", par, cc), "IDENTB"], writes=[("PS", 6)])
                if rnd == 0:
                    k.op("dve", lambda e: e.tensor_copy(out=TOK[tp][:, 0:640], in_=PS[6][:, 0:320].bitcast(BF16)),
                         reads=[("PS", 6), ("TOK", tp)], writes=[("TOK", tp)])
                else:
                    k.op("act", lambda e: e.activation(out=TOK[tp][:, 640:1280], in_=PS[6][:, 0:320].bitcast(BF16),
                                                       func=AF.Identity),
                         reads=[("PS", 6), ("TOK", tp)], writes=[("TOK", tp)])

        def state_update(ci, tp, d):
            xt3 = TOK[tp][:, 0:1024].rearrange("p (h q) -> p h q", q=64)
            k.op("dve", lambda e: e.tensor_tensor(
                out=XD[:], in0=xt3, in1=bc(WTS[:, ci, d * 16:(d + 1) * 16].unsqueeze(2), [128, 16, 64]),
                op=ALU.mult), reads=[("TOK", tp), ("WTS", ci), "XD"], writes=["XD"])
            k.op("dve", lambda e: e.tensor_tensor(
                out=HS[d][:], in0=HS[d][:], in1=bc(EGT[:, ci, d * 16:(d + 1) * 16].unsqueeze(2), [128, 16, 64]),
                op=ALU.mult), reads=[("HS", d), ("EGT", ci)], writes=[("HS", d)])
            for g in range(2):
                k.op("pe", lambda e, g=g: e.matmul(
                    PS[7][:, 0:512], lhsT=TOK[tp][:, 1024 + g * 128:1024 + (g + 1) * 128],
                    rhs=XD[:, 8 * g:8 * g + 8, :].rearrange("p h q -> p (h q)"), start=True, stop=True),
                    reads=[("TOK", tp), "XD"], writes=[("PS", 7)])
                k.op("dve", lambda e, g=g: e.tensor_tensor(
                    out=HS[d][:, 8 * g:8 * g + 8, :], in0=HS[d][:, 8 * g:8 * g + 8, :],
                    in1=PS[7][:, 0:512].rearrange("p (h q) -> p h q", q=64), op=ALU.add),
                    reads=[("HS", d), ("PS", 7)], writes=[("HS", d)])

        all_xbc = lambda par, lo, hi: [("XBC", par, c) for c in range(lo, hi)]

        k.op("pool", lambda e: e.memset(HS[1][:], 0.0), writes=[("HS", 1)])
        k.op("pool", lambda e: e.memset(HS[0][:], 0.0), writes=[("HS", 0)])
        order = [0] + list(range(NT - 1, 0, -1))
        front(order[0], 0, False)
        for it, t in enumerate(order):
            par = it % 2
            if it + 1 < len(order):
                front(order[it + 1], (it + 1) % 2, False)
            for hf in (1, 0):
                ci = 2 * t + hf
                tp = hf
                to_tok(par, hf, tp)
                hb = HSBB[ci % 2]
                k.op("act", lambda e, hb=hb: e.activation(out=hb[:], in_=HS[1][:], func=AF.Identity),
                     reads=[("HS", 1), ("HSBB", ci % 2)], writes=[("HSBB", ci % 2)])
                k.dma("sp", HBD[ci], hb[:].rearrange("p h q -> p (h q)"), reads=[("HSBB", ci % 2)],
                      writes=[("HBD", ci)])
                state_update(ci, tp, 1)

        def stage_a(ci):
            t, hf = ci // 2, ci % 2
            par, tp, cp = t % 2, hf, ci % 2
            sl = slice(hf * 128, (hf + 1) * 128)
            xb = XBC[par]
            to_tok(par, hf, tp)
            k.dma("sp", ACB[:].rearrange("p h l -> p (h l)"), GROWD[ci:ci + 1, :].partition_broadcast(128),
                  reads=["GROWD", "ACB"], writes=["ACB"])
            k.dma("sp", HSBB[cp][:].rearrange("p h q -> p (h q)"), HBD[ci], reads=[("HBD", ci), ("HSBB", cp)],
                  writes=[("HSBB", cp)])
            k.op("act", lambda e: e.activation(out=HSBF[cp][:], in_=HS[0][:], func=AF.Identity),
                 reads=[("HS", 0), ("HSBF", cp)], writes=[("HSBF", cp)])
            state_update(ci, tp, 0)
            xt3 = TOK[tp][:, 0:1024].rearrange("p (h q) -> p h q", q=64)
            k.op("dve", lambda e: e.tensor_tensor(
                out=XF[cp][:], in0=xt3, in1=bc(DT[:, ci, 0:16].unsqueeze(2), [128, 16, 64]), op=ALU.mult),
                reads=[("TOK", tp), ("DT", ci), ("XF", cp)], writes=[("XF", cp)])
            k.op("pool", lambda e: e.tensor_tensor(
                out=XB[cp][:], in0=xt3, in1=bc(DT[:, ci, 16:32].unsqueeze(2), [128, 16, 64]), op=ALU.mult),
                reads=[("TOK", tp), ("DT", ci), ("XB", cp)], writes=[("XB", cp)])
            for g in range(2):
                k.op("pe", lambda e, g=g: e.matmul(
                    PS[6][:, g * 128:(g + 1) * 128], lhsT=xb[:, 8 + g, sl], rhs=xb[:, 10 + g, sl],
                    start=True, stop=True), reads=all_xbc(par, 8, 12), writes=[("PS", 6)])
            for d, tri in enumerate((TRIF, TRIB)):
                k.op("dve", lambda e, d=d, tri=tri: e.tensor_tensor(
                    out=CBM[cp][:, 2 * d:2 * d + 2, :], in0=PS[6][:, 0:256].rearrange("p (g l) -> p g l", g=2),
                    in1=bc(tri.unsqueeze(1), [128, 2, 128]), op=ALU.mult),
                    reads=[("PS", 6), "CONST", "CBM"], writes=["CBM"])
            k.op("act", lambda e: e.activation(out=EB[:], in_=ACB[:], func=AF.Exp), reads=["ACB", "EB"],
                 writes=["EB"])
            for d in range(2):
                k.op("pool", lambda e, d=d: e.tensor_tensor(
                    out=CS[cp][:, 16 * d:16 * d + 16, :].rearrange("p (g h) l -> p g h l", g=2),
                    in0=EB[:, 16 * d:16 * d + 16, :].rearrange("p (g h) l -> p g h l", g=2),
                    in1=bc(xb[:, 10:12, sl].unsqueeze(2), [128, 2, 8, 128]), op=ALU.mult),
                    reads=["EB", ("CS", cp)] + all_xbc(par, 10, 12), writes=[("CS", cp)])
            k.op("dve", lambda e: e.tensor_tensor(
                out=ACB[:], in0=ACB[:], in1=bc(GT[:, ci, :].unsqueeze(2), [128, 32, 128]), op=ALU.subtract),
                reads=["ACB", ("GT", ci), "EB"], writes=["ACB"])
            k.op("dve", lambda e: e.tensor_scalar(out=ACB[:], in0=ACB[:], scalar1=0.0, scalar2=None,
                                                  op0=ALU.min), reads=["ACB"], writes=["ACB"])
            k.op("act", lambda e: e.activation(out=ACB[:], in_=ACB[:], func=AF.Exp), reads=["ACB"],
                 writes=["ACB"])
            for d in range(2):
                k.op("dve", lambda e, d=d: e.tensor_tensor(
                    out=WW[cp][:, 16 * d:16 * d + 16, :].rearrange("p (g h) l -> p g h l", g=2),
                    in0=ACB[:, 16 * d:16 * d + 16, :].rearrange("p (g h) l -> p g h l", g=2),
                    in1=bc(CBM[cp][:, 2 * d:2 * d + 2, :].unsqueeze(2), [128, 2, 8, 128]), op=ALU.mult),
                    reads=["ACB", "CBM", ("WW", cp)], writes=[("WW", cp)])

        def stage_b(ci):
            t, hf = ci // 2, ci % 2
            par, tp, cp = t % 2, hf, ci % 2
            sl = slice(hf * 128, (hf + 1) * 128)
            xb = XBC[par]
            for h in range(16):
                po = 64 * (h % 2)
                cc = h // 2
                pb = 2 * cp + cc // 4
                ov = PS[pb][po:po + 64, (cc % 4) * 128:(cc % 4 + 1) * 128]
                ops = [(XF[cp][:, h, :], WW[cp][:, h, :], [("XF", cp), ("WW", cp)]),
                       (XB[cp][:, h, :], WW[cp][:, 16 + h, :], [("XB", cp), ("WW", cp)]),
                       (HSBF[cp][:, h, :], CS[cp][:, h, :], [("HSBF", cp), ("CS", cp)]),
                       (HSBB[cp][:, h, :], CS[cp][:, 16 + h, :], [("HSBB", cp), ("CS", cp)])]
                for i, (lt, rh, rk) in enumerate(ops):
                    k.op("pe", lambda e, ov=ov, lt=lt, rh=rh, i=i: e.matmul(ov, lhsT=lt, rhs=rh, start=(i == 0),
                                                                               stop=(i == 3)),
                         reads=rk, writes=[("PS", pb)])
            for cc in range(KC):
                pb = 2 * cp + cc // 4
                pv = PS[pb][:, (cc % 4) * 128:(cc % 4 + 1) * 128]
                k.op("dve", lambda e, cc=cc, pv=pv: e.scalar_tensor_tensor(
                    out=YFs[:, cc, :], in0=xb[:, cc, sl], scalar=V("sdv", cc), in1=pv, op0=ALU.mult,
                    op1=ALU.add), reads=[("XBC", par, cc), ("PS", pb), "VEC", "YFs"], writes=["YFs"])
            k.op("dve", lambda e: e.tensor_tensor(out=YBs[:], in0=YFs[:], in1=SZ[par][:, :, sl], op=ALU.mult),
                 reads=["YFs", "YBs"] + [("SZ", par, c) for c in range(KC)], writes=["YBs"])
            k.dma("sp", YD[b, 0:DM, ci * 128:(ci + 1) * 128].rearrange("(c p) t -> p c t", p=128), YBs[:],
                  reads=["YBs"], writes=[("YDs", ci)])

        front(0, 0, True)
        stage_a(0)
        for ci in range(NCH):
            nx = ci + 1
            if nx < NCH:
                if nx % 2 == 0:
                    front(nx // 2, (nx // 2) % 2, True)
                stage_a(nx)
            stage_b(ci)
        k.barrier()
        S.close()

    def phase_D(l, b, last):
        S = Scope(nc)
        WO = S.sb("WO", [128, 16, DM], BF16)
        YT = [S.sb("YT%d" % i, [128, 16, TW], BF16) for i in range(2)]
        XT = [S.sb("XTd%d" % i, [128, KC, TW]) for i in range(2)]
        RB = [S.sb("RB%d" % i, [128, 2, TW]) for i in range(2)]
        T1 = [S.sb("T1_%d" % i, [128, TW]) for i in range(2)]
        T2 = [S.sb("T2_%d" % i, [128, TW]) for i in range(2)]
        SQd = S.sb("SQd", [128, 16, TW])
        load_cast(WO, w_out[l], 16, DM, "WO", scale_vec=lambda kk: V("snw", kk) if kk < 8 else V("lnw", kk - 8))
        src = xsrc_of(l)
        tl = list(range(1 if last else 0, NT))

        def loads(t):
            par = t % 2
            sl = slice(t * TW, (t + 1) * TW)
            k.dma("sp", YT[par][:], YD[b].rearrange("(c p) t -> p c t", p=128)[:, :, sl], writes=[("YT", par)])
            k.dma("sp", XT[par][:], xtile(src, b, t), writes=[("XTd", par)])

        loads(tl[0])
        for ti, t in enumerate(tl):
            par = t % 2
            col = 2 if t == 0 else b
            if ti + 1 < len(tl):
                loads(tl[ti + 1])
            k.op("act", lambda e, par=par: e.activation(out=SQd[:], in_=YT[par][:], func=AF.Square),
                 reads=[("YT", par), "SQd"], writes=["SQd"])
            for half in range(2):
                for kk in range(8):
                    k.op("pe", lambda e, kk=kk, half=half: e.matmul(
                        PS[2][:, half * TW:(half + 1) * TW], lhsT=ONES, rhs=SQd[:, half * 8 + kk, :],
                        start=(kk == 0), stop=(kk == 7)), reads=["SQd", "CONST"], writes=[("PS", 2)])
            rstd_from_ms(RB[par][:].rearrange("p a t -> p (a t)"), PS[2][:, 0:2 * TW], [("PS", 2)], [("RB", par)])
            for d in range(KC):
                pb = d % 2
                t1, t2 = T1[pb], T2[pb]
                for kk in range(16):
                    half = kk // 8
                    k.op("pe", lambda e, kk=kk, d=d, pb=pb, half=half: e.matmul(
                        PS[pb][:, half * TW:(half + 1) * TW], lhsT=WO[:, kk, d * 128:(d + 1) * 128],
                        rhs=YT[par][:, kk, :], start=(kk % 8 == 0), stop=(kk % 8 == 7)),
                        reads=["WO", ("YT", par)], writes=[("PS", pb)])
                k.op("dve", lambda e, pb=pb, t1=t1: e.tensor_tensor(out=t1[:], in0=PS[pb][:, 0:TW],
                                                                     in1=RB[par][:, 0, :], op=ALU.mult),
                     reads=[("PS", pb), ("RB", par), ("T1", pb)], writes=[("T1", pb)])
                k.op("dve", lambda e, pb=pb, t2=t2: e.tensor_tensor(out=t2[:], in0=PS[pb][:, TW:2 * TW],
                                                                     in1=RB[par][:, 1, :], op=ALU.mult),
                     reads=[("PS", pb), ("RB", par), ("T2", pb)], writes=[("T2", pb)])
                k.op("dve", lambda e, t1=t1, t2=t2: e.tensor_tensor(out=t1[:], in0=t1[:], in1=t2[:], op=ALU.add),
                     reads=[("T1", pb), ("T2", pb)], writes=[("T1", pb)])
                k.op("dve", lambda e, d=d, t1=t1: e.scalar_tensor_tensor(
                    out=XT[par][:, d, :], in0=t1[:], scalar=MODP[:, 2, d, col:col + 1], in1=XT[par][:, d, :],
                    op0=ALU.mult, op1=ALU.add), reads=[("T1", pb), ("XTd", par), "MODP"], writes=[("XTd", par)])
            k.dma("sp", xtile(XR, b, t), XT[par][:], reads=[("XTd", par)], writes=[("XR", b, t)])
        k.barrier()
        S.close()

    def phase_F(l, last):
        moe = (l % 2 == 1)
        li = l // 2
        tiles = [(b, t) for b in range(NB) for t in range(1 if last else 0, NT)]
        S = Scope(nc)
        st = {n: [S.sb("%sf%d" % (n, i), [128, KC, TW]) for i in range(2)] for n in ("XT", "SQ", "XN")}
        st["RS"] = [S.sb("RSf%d" % i, [128, TW]) for i in range(2)]
        HN = [S.sb("HN%d" % i, [128, KC, TW]) for i in range(2)]
        HNB = [S.sb("HNB%d" % i, [128, KC, TW], BF16) for i in range(2)]
        RW = S.sb("RW", [128, KC, NE])
        LG = S.sb("LG", [128, 8, NE])
        SM = S.sb("SM", [128, 8])
        CT = S.sb("CT", [NE, 128])
        if moe:
            k.dma("sp", RW[:], router_w[li].rearrange("(k p) n -> p k n", p=128), writes=["RW"])
        def load(it):
            b, t = tiles[it]
            k.dma("sp", st["XT"][it % 2][:], xtile(XR, b, t), writes=[("XT", it % 2)])

        def stage2(it):
            b, t = tiles[it]
            par = it % 2
            col = 2 if t == 0 else b
            if moe:
                norm_s2(st, par, 3, col, lambda c, par=par: (HN[par][:, c, :], [("HN", par)]),
                        extra_reads=[("HN", par)])
                k.op("dve", lambda e, par=par: e.tensor_copy(out=HNB[par][:], in_=HN[par][:]),
                     reads=[("HN", par), ("HNB", par)], writes=[("HNB", par)])
            else:
                norm_s2(st, par, 3, col, lambda c, par=par: (HNB[par][:, c, :], [("HNB", par)]),
                        extra_reads=[("HNB", par)])
            k.dma("sp", H2D.rearrange("(c p) t -> p c t", p=128)[:, :, b * T + t * TW:b * T + (t + 1) * TW],
                  HNB[par][:], reads=[("HNB", par)], writes=["H2D"])
            if moe:
                for hf in range(2):
                    for c in range(KC):
                        k.op("pe", lambda e, c=c, hf=hf, par=par: e.matmul(
                            PS[2][:, 0:NE], lhsT=HN[par][:, c, hf * 128:(hf + 1) * 128], rhs=RW[:, c, :],
                            start=(c == 0), stop=(c == KC - 1)), reads=[("HN", par), "RW"], writes=[("PS", 2)])
                    L0, EQ1, L2, EQ2, CB_ = (LG[:, i, :] for i in range(5))
                    M1, M2, DD, P1, P2 = (SM[:, i:i + 1] for i in range(5))
                    seq = [
                        ("dve", lambda e: e.tensor_copy(out=L0, in_=PS[2][:, 0:NE]), [("PS", 2)]),
                        ("dve", lambda e: e.tensor_reduce(out=M1, in_=L0, axis=AX.X, op=ALU.max), []),
                        ("dve", lambda e: e.tensor_scalar(out=EQ1, in0=L0, scalar1=M1, scalar2=None,
                                                          op0=ALU.is_equal), []),
                        ("dve", lambda e: e.scalar_tensor_tensor(out=L2, in0=EQ1, scalar=-1e30, in1=L0,
                                                                 op0=ALU.mult, op1=ALU.add), []),
                        ("dve", lambda e: e.tensor_reduce(out=M2, in_=L2, axis=AX.X, op=ALU.max), []),
                        ("dve", lambda e: e.tensor_scalar(out=EQ2, in0=L2, scalar1=M2, scalar2=None,
                                                          op0=ALU.is_equal), []),
                        ("dve", lambda e: e.tensor_tensor(out=DD, in0=M2, in1=M1, op=ALU.subtract), []),
                        ("act", lambda e: e.activation(out=DD, in_=DD, func=AF.Exp), []),
                        ("dve", lambda e: e.tensor_scalar(out=P1, in0=DD, scalar1=1.0, scalar2=None, op0=ALU.add),
                         []),
                        ("dve", lambda e: e.reciprocal(out=P1, in_=P1), []),
                        ("dve", lambda e: e.tensor_tensor(out=P2, in0=DD, in1=P1, op=ALU.mult), []),
                        ("dve", lambda e: e.tensor_scalar(out=CB_, in0=EQ1, scalar1=P1, scalar2=None,
                                                          op0=ALU.mult), []),
                        ("dve", lambda e: e.scalar_tensor_tensor(out=CB_, in0=EQ2, scalar=P2, in1=CB_,
                                                                 op0=ALU.mult, op1=ALU.add), []),
                    ]
                    for eng, fn, rk in seq:
                        k.op(eng, fn, reads=["LG"] + rk, writes=["LG"])
                    k.op("pe", lambda e: e.transpose(PS[3][0:NE, 0:128], CB_, IDENTF), reads=["LG", "CONST"],
                         writes=[("PS", 3)])
                    k.op("dve", lambda e: e.tensor_copy(out=CT[:], in_=PS[3][0:NE, 0:128]),
                         reads=[("PS", 3), "CT"], writes=["CT"])
                    o0 = b * T + t * TW + hf * 128
                    k.dma("sp", CWD[:, o0:o0 + 128], CT[:], reads=["CT"], writes=["CWD"])

        norm_pipeline(st, len(tiles), load, stage2)
        k.barrier()
        S.close()
        S = Scope(nc)
        W1 = S.sb("W1", [128, KC, DFF], BF16)
        W3 = S.sb("W3", [128, KC, DFF], BF16)
        W2 = S.sb("W2", [128, FC, DM], BF16)
        H2T = [S.sb("H2T%d" % i, [128, KC, TW], BF16) for i in range(2)]
        XT = [S.sb("XTe%d" % i, [128, KC, TW]) for i in range(2)]
        CWB = [S.sb("CWB%d" % i, [128, TW]) for i in range(2)]
        SG = [S.sb("SG%d" % i, [128, TW]) for i in range(2)]
        AV = S.sb("AVall", [128, FC, TW], BF16)
        T1 = [S.sb("T1e%d" % i, [128, TW]) for i in range(2)]
        stg_extra.extend([S.sb("STGF%d" % i, [128, 1024]) for i in range(4)])
        for ex in range(NE if moe else 1):
            if moe:
                s1, s3, s2 = moe_w1[li, ex], moe_w3[li, ex], moe_w2[li, ex]
            else:
                s1, s3, s2 = ffn_w1[li], ffn_w3[li], ffn_w2[li]
            load_cast(W1, s1, KC, DFF, "W1")
            load_cast(W3, s3, KC, DFF, "W3")
            load_cast(W2, s2, FC, DM, "W2")
            def loads(it, ex=ex):
                b, t = tiles[it]
                par = it % 2
                o0 = b * T + t * TW
                k.dma("sp", H2T[par][:], H2D.rearrange("(c p) t -> p c t", p=128)[:, :, o0:o0 + TW],
                      reads=["H2D"], writes=[("H2T", par)])
                k.dma("sp", XT[par][:], xtile(XR, b, t), reads=[("XR", b, t)], writes=[("XTe", par)])
                if moe:
                    k.dma("sp", CWB[par][:], CWD[ex:ex + 1, o0:o0 + TW].partition_broadcast(128), reads=["CWD"],
                          writes=[("CWB", par)])

            loads(0)
            for it, (b, t) in enumerate(tiles):
                par = it % 2
                col = 2 if t == 0 else b
                o0 = b * T + t * TW
                if it + 1 < len(tiles):
                    loads(it + 1)
                for f in range(FC):
                    fp = f % 2
                    pg, pu = 4 + fp, 6 + fp
                    for kk in range(KC):
                        k.op("pe", lambda e, kk=kk, f=f, pg=pg: e.matmul(
                            PS[pg][:, 0:TW], lhsT=W1[:, kk, f * 128:(f + 1) * 128], rhs=H2T[par][:, kk, :],
                            start=(kk == 0), stop=(kk == KC - 1)), reads=["W1", ("H2T", par)],
                            writes=[("PS", pg)])
                    for kk in range(KC):
                        k.op("pe", lambda e, kk=kk, f=f, pu=pu: e.matmul(
                            PS[pu][:, 0:TW], lhsT=W3[:, kk, f * 128:(f + 1) * 128], rhs=H2T[par][:, kk, :],
                            start=(kk == 0), stop=(kk == KC - 1)), reads=["W3", ("H2T", par)],
                            writes=[("PS", pu)])
                    k.op("act", lambda e, fp=fp, pg=pg: e.activation(out=SG[fp][:], in_=PS[pg][:, 0:TW],
                                                                      func=AF.Silu), reads=[("PS", pg)],
                         writes=[("SG", fp)])
                    k.op("dve", lambda e, fp=fp, pu=pu, f=f: e.tensor_tensor(out=AV[:, f, :], in0=SG[fp][:],
                                                                              in1=PS[pu][:, 0:TW], op=ALU.mult),
                         reads=[("SG", fp), ("PS", pu)], writes=[("AV", f)])
                for d in range(KC):
                    dp = d % 2
                    for f in range(FC):
                        k.op("pe", lambda e, d=d, f=f, dp=dp: e.matmul(
                            PS[dp][:, 0:TW], lhsT=W2[:, f, d * 128:(d + 1) * 128], rhs=AV[:, f, :],
                            start=(f == 0), stop=(f == FC - 1)), reads=["W2", ("AV", f)], writes=[("PS", dp)])
                    pv = PS[dp][:, 0:TW]
                    if moe:
                        k.op("dve", lambda e, pv=pv, dp=dp: e.tensor_tensor(out=T1[dp][:], in0=pv, in1=CWB[par][:],
                                                                             op=ALU.mult),
                             reads=[("PS", dp), ("CWB", par), ("T1e", dp)], writes=[("T1e", dp)])
                        src_ap, rk = T1[dp][:], [("T1e", dp)]
                    else:
                        src_ap, rk = pv, [("PS", dp)]
                    k.op("dve", lambda e, d=d, src_ap=src_ap: e.scalar_tensor_tensor(
                        out=XT[par][:, d, :], in0=src_ap, scalar=MODP[:, 5, d, col:col + 1], in1=XT[par][:, d, :],
                        op0=ALU.mult, op1=ALU.add), reads=rk + [("XTe", par), "MODP"], writes=[("XTe", par)])
                k.dma("sp", xtile(XR, b, t), XT[par][:], reads=[("XTe", par)], writes=[("XR", b, t)])
        k.barrier()
        del stg_extra[:]
        S.close()

    def phase_out():
        S = Scope(nc)
        st = {n: [S.sb("%so%d" % (n, i), [128, KC, TW]) for i in range(2)] for n in ("XT", "SQ", "XN")}
        st["RS"] = [S.sb("RSo%d" % i, [128, TW]) for i in range(2)]
        tiles = [(b, t) for b in range(NB) for t in range(1, NT)]

        def load(it):
            b, t = tiles[it]
            k.dma("sp", st["XT"][it % 2][:], xtile(XR, b, t), writes=[("XT", it % 2)])

        def stage2(it):
            b, t = tiles[it]
            par = it % 2
            XN = st["XN"][par]
            k.op("dve", lambda e: e.tensor_tensor(out=XN[:], in0=XN[:], in1=bc(V("fnw", 0, 8).unsqueeze(2),
                                                                                [128, KC, TW]), op=ALU.mult),
                 reads=[("XN", par), "VEC"], writes=[("XN", par)])
            k.dma("sp", out[b].rearrange("(c p) t -> p c t", p=128)[:, :, (t - 1) * TW:t * TW], XN[:],
                  reads=[("XN", par)], writes=["OUT"])

        norm_pipeline(st, len(tiles), load, stage2)
        k.barrier()
        S.close()

    for l in range(nlayers):
        last = (l == 1)
        layer_setup(l)
        k.barrier()
        for b in range(NB):
            if "A" in phases:
                SH = Scope(nc)
                HT = SH.sb("HT", [128, KC, T], BF16)
                phase_A(l, b, HT)
                if "B" in phases:
                    phase_B(l, b, HT)
                k.barrier()
                SH.close()
            if "C" in phases:
                phase_C(l, b)
            if "D" in phases:
                phase_D(l, b, last)
        if "F" in phases:
            phase_F(l, last)
    if nlayers == 2 and "F" in phases:
        phase_out()
    k.barrier()
    return nc, k


def _pack_vecs(inp, l):
    def fm(v):
        v = np.asarray(v, np.float32)
        return v.reshape(-1, 128).T

    cols = [fm(inp["norm1_w"][l]), fm(inp["norm2_w"][l]), fm(inp["mod_b"][l])]
    cols += [fm(inp["ssd_conv_w"][l][tap]) for tap in range(4)]
    cols += [fm(inp["ssd_conv_b"][l]), fm(inp["ssd_norm_w"][l])]
    cols += [fm(inp["lru_conv_w"][l][tap]) for tap in range(4)]
    cols += [fm(inp["lru_conv_b"][l])]
    cols += [fm(inp["lru_rb"][l][d]) for d in range(2)]
    cols += [fm(inp["lru_ib"][l][d]) for d in range(2)]
    cols += [fm(inp["lru_lambda"][l][d]) for d in range(2)]
    cols += [fm(inp["lru_norm_w"][l]), fm(np.repeat(np.asarray(inp["ssd_d"][l], np.float32), 64)),
             fm(inp["final_norm_w"])]
    out = np.concatenate(cols, axis=1)
    assert out.shape == (128, NV), out.shape
    return out


def _consts():
    c = np.zeros((128, 4, 128), np.float32)
    c[:, 0, :] = 1.0
    kk = np.arange(128)[:, None]
    ll = np.arange(128)[None, :]
    c[:, 1, :] = (kk <= ll)
    c[:, 2, :] = (kk >= ll)
    c[:, 3, :] = (kk == ll)
    return c


def make_in_maps(inp, cores=range(NCORES), lite=False):
    f = lambda a: np.ascontiguousarray(np.asarray(a, dtype=np.float32))
    vecs = np.stack([_pack_vecs(inp, l) for l in range(2)], 0)
    dtrow = np.stack([np.stack([np.asarray(inp["ssd_dt_bias"][l]).reshape(32),
                                np.asarray(inp["ssd_a_log"][l]).reshape(32)], 0) for l in range(2)], 0)
    shared = {"vecs": f(vecs), "consts": _consts(), "dtrow": f(dtrow)}
    for n in ("mod_w", "w_in", "lru_rw", "lru_iw", "w_out", "ffn_w1", "ffn_w3", "ffn_w2", "router_w", "moe_w1",
              "moe_w3", "moe_w2"):
        if lite and n.startswith("moe_w"):
            continue
        shared[n] = f(inp[n])
    maps = []
    for i in cores:
        bs = [NB * i + j for j in range(NB)]
        xin = np.stack([np.concatenate([inp["ctx"][b], inp["x"][b]], 0).T for b in bs], 0)
        cv = np.zeros((128, KC, 4), np.float32)
        for j, b in enumerate(bs):
            cv[:, :, j] = np.asarray(inp["c"][b], np.float32).reshape(KC, 128).T
        cv[:, :, 2] = np.asarray(inp["c_ctx"], np.float32).reshape(KC, 128).T
        m = dict(shared)
        m["xin"] = f(xin)
        m["cvec"] = cv
        maps.append(m)
    return maps


def kernel(**inputs):
    nc, _ = build()
    maps = make_in_maps(inputs)
    res = run_bass_kernel_spmd(nc, maps, core_ids=list(range(NCORES)))
    outs = []
    for i in range(NCORES):
        o = np.asarray(res.results[i]["out"])
        outs.append(np.transpose(o, (0, 2, 1)))
    return np.ascontiguousarray(np.concatenate(outs, 0).astype(np.float32))
```

```python
import numpy as np
import concourse.bass as bass
import concourse.mybir as mybir
from concourse.bass_utils import run_bass_kernel_spmd

F32 = mybir.dt.float32
BF16 = mybir.dt.bfloat16
AF = mybir.ActivationFunctionType
ALU = mybir.AluOpType
AX = mybir.AxisListType

SAME_ENGINE_SYNC = True
N_DMA_SEMS = 12

NCORES = 8
NB = 2
T = 4352
TW = 256
NT = 17
DM = 1024
KC = 8
OFF_XBC = 1024
OFF_DT = 2560
OFF_LX = 2592
OFF_LG = 3616
DIN = 4640
DFF = 2816
FC = 22
NE = 8
EPS = 1e-6
NCH = 34

VOFF = {}
_o = 0
for _n, _w in [("n1w", 8), ("n2w", 8), ("modb", 48), ("scw", 48), ("scb", 12), ("snw", 8), ("lcw", 32), ("lcb", 8),
               ("lrb", 16), ("lib", 16), ("llam", 16), ("lnw", 8), ("sdv", 8), ("fnw", 8)]:
    VOFF[_n] = _o
    _o += _w
NV = _o


class K:
    def __init__(self, nc):
        self.nc = nc
        self.engs = {"pe": nc.tensor, "act": nc.scalar, "dve": nc.vector, "pool": nc.gpsimd, "sp": nc.sync}
        self.sem = {e: nc.semaphore("sem_" + e).__enter__() for e in self.engs}
        self.cnt = {e: 0 for e in self.engs}
        self.seen = {e: {} for e in self.engs}
        self.lw = {}
        self.rd = {}
        self.dsem = {}
        self.dcnt = {}
        self.dnext = {}
        for q in ("sp", "pool"):
            self.dsem[q] = [nc.semaphore("dsem_%s_%d" % (q, i)).__enter__() for i in range(N_DMA_SEMS)]
            self.dcnt[q] = [0] * N_DMA_SEMS
            self.dnext[q] = 0
        self.n_inst = 0

    def _semof(self, o):
        if isinstance(o, tuple):
            return self.dsem[o[1]][o[2]]
        return self.sem[o]

    def _wait(self, e, o, v):
        if o == e and (not SAME_ENGINE_SYNC or e == "pe"):
            return
        if self.seen[e].get(o, 0) >= v:
            return
        self.engs[e].wait_ge(self._semof(o), v)
        self.seen[e][o] = v

    def _deps(self, e, reads, writes):
        need = {}
        for k in reads:
            w = self.lw.get(k)
            if w is not None and need.get(w[0], 0) < w[1]:
                need[w[0]] = w[1]
        for k in writes:
            w = self.lw.get(k)
            if w is not None and need.get(w[0], 0) < w[1]:
                need[w[0]] = w[1]
            for o, v in self.rd.get(k, {}).items():
                if need.get(o, 0) < v:
                    need[o] = v
        for o, v in need.items():
            self._wait(e, o, v)

    def _record(self, who, v, reads, writes):
        for k in reads:
            self.rd.setdefault(k, {})[who] = v
        for k in writes:
            self.lw[k] = (who, v)
            self.rd[k] = {}

    def op(self, e, emit, reads=(), writes=()):
        self._deps(e, reads, writes)
        inst = emit(self.engs[e])
        self.cnt[e] += 1
        inst.then_inc(self.sem[e], 1)
        self._record(e, self.cnt[e], reads, writes)
        self.n_inst += 1
        return inst

    def dma(self, q, out, in_, reads=(), writes=(), **kw):
        j = self.dnext[q]
        self.dnext[q] = (j + 1) % N_DMA_SEMS
        who = ("dma", q, j)
        if self.dcnt[q][j] > 0:
            self._wait(q, who, self.dcnt[q][j])
        self._deps(q, reads, writes)
        inst = self.engs[q].dma_start(out=out, in_=in_, **kw)
        self.dcnt[q][j] += 16
        inst.then_inc(self.dsem[q][j], 16)
        self._record(who, self.dcnt[q][j], reads, writes)
        self.n_inst += 1
        return inst

    def barrier(self):
        for e in self.engs:
            for o in self.engs:
                if self.cnt[o] > 0:
                    self._wait(e, o, self.cnt[o])
            for q in self.dsem:
                for j in range(N_DMA_SEMS):
                    if self.dcnt[q][j] > 0:
                        self._wait(e, ("dma", q, j), self.dcnt[q][j])
        self.lw = {}
        self.rd = {}


class Scope:
    def __init__(self, nc):
        self.nc = nc
        self.guards = []

    _uid = [0]

    def sb(self, name, shape, dt=F32):
        Scope._uid[0] += 1
        g = self.nc.sbuf_tensor("%s_u%d" % (name, Scope._uid[0]), shape, dt)
        t = g.__enter__()
        self.guards.append(g)
        return t

    def close(self):
        for g in reversed(self.guards):
            g.__exit__(None, None, None)
        self.guards = []


def bc(ap, shape):
    return ap.broadcast_to(shape)


def build(nlayers=2, dbg=None, phases=("A", "B", "C", "D", "F"), lite=False):
    nc = bass.Bass("TRN2", target_bir_lowering=False)
    k = K(nc)
    dbg = dbg or {}

    def din(name, shape, dt=F32):
        return nc.dram_tensor(name, list(shape), dt, kind="ExternalInput").ap()

    def dint(name, shape, dt=F32):
        kind = "ExternalOutput" if name in dbg else "Internal"
        return nc.dram_tensor(name, list(shape), dt, kind=kind).ap()

    xin = din("xin", [NB, DM, T])
    cvec = din("cvec", [128, KC, 4])
    vecs = din("vecs", [2, 128, NV])
    consts = din("consts", [128, 4, 128])
    dtrow = din("dtrow", [2, 2, 32])
    mod_w = din("mod_w", [2, DM, 6 * DM])
    w_in = din("w_in", [2, DM, DIN])
    lru_rw = din("lru_rw", [2, 2, 16, 64, 64])
    lru_iw = din("lru_iw", [2, 2, 16, 64, 64])
    w_out = din("w_out", [2, 2 * DM, DM])
    ffn_w1 = din("ffn_w1", [1, DM, DFF])
    ffn_w3 = din("ffn_w3", [1, DM, DFF])
    ffn_w2 = din("ffn_w2", [1, DFF, DM])
    router_w = din("router_w", [1, DM, NE])
    if lite:
        moe_w1 = moe_w3 = moe_w2 = None
    else:
        moe_w1 = din("moe_w1", [1, NE, DM, DFF])
        moe_w3 = din("moe_w3", [1, NE, DM, DFF])
        moe_w2 = din("moe_w2", [1, NE, DFF, DM])
    out = nc.dram_tensor("out", [NB, DM, 4096], F32, kind="ExternalOutput").ap()

    XR = dint("XR", [NB, DM, T])
    HTD = dint("HTD", [NB, DM, T], BF16)
    YD = dint("YD", [NB, 2 * DM, T], BF16)
    RSD = dint("RSD", [NB, 2, T])
    H2D = dint("H2D", [DM, NB * T], BF16)
    CWD = dint("CWD", [NE, NB * T])
    GROWD = dint("GROWD", [NCH, 32 * 128])
    HBD = dint("HBD", [NCH, 128, DM], BF16)
    DBGB = dint("DBGB", [8, 128, T]) if "DBGB" in dbg else None

    P = Scope(nc)
    CONST = P.sb("CONST", [128, 4, 128])
    ONES = CONST[:, 0, :]
    TRIF = CONST[:, 1, :]
    TRIB = CONST[:, 2, :]
    IDENTF = CONST[:, 3, :]
    IDENTB = P.sb("IDENTB", [128, 128], BF16)
    CV = P.sb("CV", [128, KC, 4])
    SC = P.sb("SC", [128, KC, 4])
    VEC = P.sb("VEC", [128, NV])
    MODV = P.sb("MODV", [128, 48, 4])
    MODP = P.sb("MODP", [128, 6, KC, 4])
    SCL = P.sb("SCL", [128, 16])
    SCL2 = P.sb("SCL2", [128, 16])
    STG = [P.sb("STG%d" % i, [128, 1024]) for i in range(2)]
    PS = [nc.psum_tensor("PS%d" % i, [128, 512], F32).__enter__() for i in range(8)]
    stg_i = [0]

    def V(name, i=0, n=1):
        o = VOFF[name] + i
        return VEC[:, o:o + n]

    k.dma("sp", CONST[:], consts[:, :, :], writes=["CONST"])
    k.op("pool", lambda e: e.tensor_copy(out=IDENTB[:], in_=IDENTF), reads=["CONST"], writes=["IDENTB"])
    k.dma("sp", CV[:], cvec[:, :, :], writes=["CV"])
    k.op("act", lambda e: e.activation(out=SC[:], in_=CV[:], func=AF.Silu), reads=["CV"], writes=["SC"])

    stg_extra = []

    def load_cast(dst, src2d, kc, ncols, key, scale_vec=None, q="sp"):
        pc = min(ncols, 1024)
        while ncols % pc != 0:
            pc -= 1
        srcv = src2d.rearrange("(k p) n -> p k n", p=128)
        keyf = key if callable(key) else (lambda kk, c0: key)
        order = [(kk, c0) for c0 in range(0, ncols, pc) for kk in range(kc)] if callable(key) else \
                [(kk, c0) for kk in range(kc) for c0 in range(0, ncols, pc)]
        for kk, c0 in order:
            if True:
                key = keyf(kk, c0)
                bufs = STG + stg_extra
                si = stg_i[0] % len(bufs)
                stg_i[0] += 1
                sv = bufs[si][:, 0:pc]
                k.dma(q, sv, srcv[:, kk, c0:c0 + pc], writes=[("STG", si)])
                eng = "dve" if (stg_i[0] % 4) < 2 else "act"
                sc = None if scale_vec is None else scale_vec(kk)
                rk = [("STG", si)] + ([] if sc is None else ["VEC"])
                if eng == "dve":
                    if sc is None:
                        k.op("dve", lambda e, sv=sv, c0=c0, kk=kk: e.tensor_copy(out=dst[:, kk, c0:c0 + pc], in_=sv),
                             reads=rk, writes=[key])
                    else:
                        k.op("dve", lambda e, sv=sv, c0=c0, kk=kk, sc=sc: e.tensor_scalar(
                            out=dst[:, kk, c0:c0 + pc], in0=sv, scalar1=sc, scalar2=None, op0=ALU.mult),
                            reads=rk, writes=[key])
                else:
                    if sc is None:
                        k.op("act", lambda e, sv=sv, c0=c0, kk=kk: e.activation(out=dst[:, kk, c0:c0 + pc], in_=sv,
                                                                                 func=AF.Identity),
                             reads=rk, writes=[key])
                    else:
                        k.op("act", lambda e, sv=sv, c0=c0, kk=kk, sc=sc: e.activation(
                            out=dst[:, kk, c0:c0 + pc], in_=sv, func=AF.Identity, scale=sc), reads=rk, writes=[key])

    def rstd_from_ms(eng_out, src, key_r, key_w):
        k.op("act", lambda e: e.activation(out=eng_out, in_=src, func=AF.Sqrt, scale=1.0 / DM, bias=EPSV[:, 0:1]),
             reads=key_r + ["EPSV"], writes=key_w)
        k.op("dve", lambda e: e.reciprocal(out=eng_out, in_=eng_out), reads=key_w, writes=key_w)

    EPSV = P.sb("EPSV", [128, 2])
    k.op("dve", lambda e: e.memset(EPSV[:, 0:1], EPS), writes=["EPSV"])
    k.op("dve", lambda e: e.memset(EPSV[:, 1:2], 1.0), reads=["EPSV"], writes=["EPSV"])

    def layer_setup(l):
        k.dma("sp", VEC[:], vecs[l, :, :], writes=["VEC"])
        for pc in range(48):
            si = stg_i[0] % 2
            stg_i[0] += 1
            sv = STG[si][:, :].rearrange("p (k n) -> p k n", k=KC)
            k.dma("sp", sv, mod_w[l].rearrange("(k p) n -> p k n", p=128)[:, :, pc * 128:(pc + 1) * 128],
                  writes=[("STG", si)])
            for h in range(1):
                oc = pc + h
                for kk in range(KC):
                    k.op("pe", lambda e, kk=kk, h=h, sv=sv: e.matmul(
                        PS[0][:, h * 4:h * 4 + 4], lhsT=sv[:, kk, h * 128:(h + 1) * 128], rhs=SC[:, kk, :],
                        start=(kk == 0), stop=(kk == KC - 1)), reads=[("STG", si), "SC"], writes=[("PS", 0)])
                k.op("dve", lambda e, oc=oc, h=h: e.tensor_scalar(
                    out=MODV[:, oc, :], in0=PS[0][:, h * 4:h * 4 + 4], scalar1=V("modb", oc), scalar2=None,
                    op0=ALU.add), reads=[("PS", 0), "VEC"], writes=["MODV"])
        for s, (nwn, ish, isc, igt) in enumerate([("n1w", 0, 1, 2), ("n2w", 3, 4, 5)]):
            k.op("dve", lambda e, s=s, isc=isc: e.tensor_scalar(
                out=MODP[:, 3 * s, :, :], in0=MODV[:, isc * 8:isc * 8 + 8, :], scalar1=1.0, scalar2=None,
                op0=ALU.add), reads=["MODV"], writes=["MODP"])
            k.op("dve", lambda e, s=s, nwn=nwn: e.tensor_tensor(
                out=MODP[:, 3 * s, :, :], in0=MODP[:, 3 * s, :, :],
                in1=bc(V(nwn, 0, 8).unsqueeze(2), [128, 8, 4]), op=ALU.mult), reads=["MODP", "VEC"], writes=["MODP"])
            k.op("dve", lambda e, s=s, ish=ish: e.tensor_copy(
                out=MODP[:, 3 * s + 1, :, :], in_=MODV[:, ish * 8:ish * 8 + 8, :]), reads=["MODV"], writes=["MODP"])
            k.op("dve", lambda e, s=s, igt=igt: e.tensor_copy(
                out=MODP[:, 3 * s + 2, :, :], in_=MODV[:, igt * 8:igt * 8 + 8, :]), reads=["MODV"], writes=["MODP"])
        k.op("act", lambda e: e.activation(out=SCL[:], in_=V("llam", 0, 16), func=AF.Exp, scale=-1.0),
             reads=["VEC"], writes=["SCL"])
        k.op("act", lambda e: e.activation(out=SCL[:], in_=SCL[:], func=AF.Ln, bias=EPSV[:, 1:2]),
             reads=["SCL", "EPSV"], writes=["SCL"])
        k.op("dve", lambda e: e.tensor_scalar(out=SCL2[:], in0=SCL[:], scalar1=-16.0, scalar2=None, op0=ALU.mult),
             reads=["SCL"], writes=["SCL2"])
        k.op("dve", lambda e: e.tensor_scalar(out=SCL[:], in0=SCL[:], scalar1=-8.0, scalar2=None, op0=ALU.mult),
             reads=["SCL", "SCL2"], writes=["SCL"])

    def xsrc_of(l):
        return xin if l == 0 else XR

    def xtile(src, b, t):
        return src[b].rearrange("(c p) t -> p c t", p=128)[:, :, t * TW:(t + 1) * TW]

    def norm_s1(S, par, psb):
        XT, SQ, RS, XN = S["XT"][par], S["SQ"][par], S["RS"][par], S["XN"][par]
        k.op("act", lambda e: e.activation(out=SQ[:], in_=XT[:], func=AF.Square), reads=[("XT", par)],
             writes=[("SQ", par)])
        for c in range(KC):
            k.op("pe", lambda e, c=c: e.matmul(PS[psb][:, 0:TW], lhsT=ONES, rhs=SQ[:, c, :], start=(c == 0),
                                               stop=(c == KC - 1)), reads=[("SQ", par), "CONST"], writes=[("PS", psb)])
        rstd_from_ms(RS[:], PS[psb][:, 0:TW], [("PS", psb)], [("RS", par)])
        k.op("dve", lambda e: e.tensor_tensor(out=XN[:], in0=XT[:], in1=bc(RS[:].unsqueeze(1), [128, KC, TW]),
                                              op=ALU.mult), reads=[("XT", par), ("RS", par), ("XN", par)],
             writes=[("XN", par)])

    def norm_s2(S, par, mslot, col, dst_fn, extra_reads=()):
        XN = S["XN"][par]
        for c in range(KC):
            dst, wkeys = dst_fn(c)
            k.op("act", lambda e, c=c, dst=dst: e.activation(
                out=dst, in_=XN[:, c, :], func=AF.Identity, scale=MODP[:, mslot, c, col:col + 1],
                bias=MODP[:, mslot + 1, c, col:col + 1]), reads=[("XN", par), "MODP"] + list(extra_reads),
                writes=wkeys)

    def norm_pipeline(S, n, load, stage2):
        load(0)
        if n > 1:
            load(1)
        for i in range(n):
            norm_s1(S, i % 2, i % 2)
            if i + 2 < n:
                load(i + 2)
            if i >= 1:
                stage2(i - 1)
        stage2(n - 1)

    def phase_A(l, b, HT):
        S = Scope(nc)
        st = {n: [S.sb("%s%d" % (n, i), [128, KC, TW]) for i in range(2)] for n in ("XT", "SQ", "XN")}
        st["RS"] = [S.sb("RS%d" % i, [128, TW]) for i in range(2)]
        src = xsrc_of(l)
        def load(t):
            k.dma("sp", st["XT"][t % 2][:], xtile(src, b, t), reads=[("XR", b, t)], writes=[("XT", t % 2)])

        def stage2(t):
            col = 2 if t == 0 else b
            norm_s2(st, t % 2, 0, col, lambda c, t=t: (HT[:, c, t * TW:(t + 1) * TW], [("HT", t)]))
            k.dma("sp", xtile(HTD, b, t), HT[:, :, t * TW:(t + 1) * TW], reads=[("HT", t)], writes=[("HTD", t)])

        norm_pipeline(st, NT, load, stage2)
        k.barrier()
        S.close()

    def phase_B(l, b, HT):
        S = Scope(nc)
        XL = S.sb("XL", [128, T])
        XC = S.sb("XC", [128, T])
        RR = S.sb("RR", [128, T])
        II = S.sb("II", [128, T])
        HF = S.sb("HF", [128, T])
        WLX = S.sb("WLX", [128, KC, 128], BF16)
        WLG = S.sb("WLG", [128, KC, 128], BF16)
        BDS = S.sb("BDS", [128, 4, 128])
        GE = [S.sb("GE%d" % i, [128, TW]) for i in range(2)]
        YB = [S.sb("YBl%d" % i, [128, TW], BF16) for i in range(2)]
        k.op("dve", lambda e: e.memset(BDS[:], 0.0), writes=["BDS"])
        pieces = [(0, 256)] + [(256 + i * 512, 256 + (i + 1) * 512) for i in range(8)]
        NP = len(pieces)
        xl_keys = ["XL"] + [("XLp", p) for p in range(NP)]
        hf_keys = ["HF"] + [("HFp", p) for p in range(NP)]
        rr_keys = [("RR", p) for p in range(NP)]
        ii_keys = [("II", p) for p in range(NP)]

        def scan_view(buf, t):
            r0 = (t - 1) * 4
            return buf[:, 256:].rearrange("p (c r) -> p r c", r=64)[:, r0:r0 + 4, :]

        def seg(buf, sgi, lo, hi):
            if sgi == 0:
                return buf[:, 0:256].rearrange("p (a r) -> p a r", a=1)[:, :, lo:hi]
            return buf[:, 256:].rearrange("p (c r) -> p c r", r=64)[:, :, lo:hi]

        def load_wlx(j):
            load_cast(WLX, w_in[l][:, OFF_LX + j * 128:OFF_LX + (j + 1) * 128], KC, 128, "WLX")

        def load_wlg(j):
            load_cast(WLG, w_in[l][:, OFF_LG + j * 128:OFF_LG + (j + 1) * 128], KC, 128, "WLG")

        def load_bd(j):
            for q, (wt, d) in enumerate([(lru_rw, 0), (lru_iw, 0), (lru_rw, 1), (lru_iw, 1)]):
                for h in range(2):
                    k.dma("sp", BDS[h * 64:(h + 1) * 64, q, h * 64:(h + 1) * 64], wt[l, d, 2 * j + h, :, :],
                          reads=["BDS"], writes=["BDS"])

        load_wlx(0)
        load_bd(0)
        for j in range(KC):
            load_wlg(j)
            for t in range(NT):
                pb = t % 2
                for kk in range(KC):
                    k.op("pe", lambda e, kk=kk, t=t, pb=pb: e.matmul(
                        PS[pb][:, 0:TW], lhsT=WLX[:, kk, :], rhs=HT[:, kk, t * TW:(t + 1) * TW], start=(kk == 0),
                        stop=(kk == KC - 1)), reads=["WLX", ("HT", t)], writes=[("PS", pb)])
                if t == 0:
                    k.op("dve", lambda e, pb=pb: e.tensor_copy(out=XL[:, 0:256], in_=PS[pb][:, 0:TW]),
                         reads=[("PS", pb)] + xl_keys, writes=xl_keys)
                else:
                    k.op("act" if t % 2 == 0 else "dve",
                         (lambda e, pb=pb, t=t: e.activation(
                             out=scan_view(XL, t), in_=PS[pb][:, 0:TW].rearrange("p (r c) -> p r c", c=64),
                             func=AF.Identity)) if t % 2 == 0 else
                         (lambda e, pb=pb, t=t: e.tensor_copy(
                             out=scan_view(XL, t), in_=PS[pb][:, 0:TW].rearrange("p (r c) -> p r c", c=64))),
                         reads=[("PS", pb)] + xl_keys, writes=xl_keys)
            if j + 1 < KC:
                load_wlx(j + 1)
            k.op("dve", lambda e, j=j: e.tensor_scalar(out=XC[:], in0=XL[:], scalar1=V("lcw", 2 * 8 + j),
                                                        scalar2=V("lcb", j), op0=ALU.mult, op1=ALU.add),
                 reads=xl_keys + ["VEC", "XC"], writes=["XC"])
            for tap, sh in [(0, -2), (1, -1), (3, 1)]:
                for sgi in range(2):
                    L = 256 if sgi == 0 else 64
                    if sh < 0:
                        o_lo, o_hi, i_lo, i_hi = -sh, L, 0, L + sh
                    else:
                        o_lo, o_hi, i_lo, i_hi = 0, L - sh, sh, L
                    ov = seg(XC, sgi, o_lo, o_hi)
                    iv = seg(XL, sgi, i_lo, i_hi)
                    k.op("dve", lambda e, ov=ov, iv=iv, tap=tap, j=j: e.scalar_tensor_tensor(
                        out=ov, in0=iv, scalar=V("lcw", tap * 8 + j), in1=ov, op0=ALU.mult, op1=ALU.add),
                        reads=xl_keys + ["XC", "VEC"], writes=["XC"])
            for d in range(2):
                order = list(range(NP)) if d == 0 else [0] + list(range(NP - 1, 0, -1))
                def stage_x(oi, p, d=d):
                    lo, hi = pieces[p]
                    w = hi - lo
                    sl = slice(lo, hi)
                    pr, pi = 2 + 2 * (oi % 2), 3 + 2 * (oi % 2)
                    k.op("pe", lambda e, pr=pr, sl=sl, d=d, w=w: e.matmul(
                        PS[pr][:, 0:w], lhsT=BDS[:, 2 * d, :], rhs=XC[:, sl], start=True, stop=True),
                        reads=["BDS", "XC"], writes=[("PS", pr)])
                    k.op("pe", lambda e, pi=pi, sl=sl, d=d, w=w: e.matmul(
                        PS[pi][:, 0:w], lhsT=BDS[:, 2 * d + 1, :], rhs=XC[:, sl], start=True, stop=True),
                        reads=["BDS", "XC"], writes=[("PS", pi)])
                    k.op("act", lambda e, pr=pr, sl=sl, d=d, j=j, w=w: e.activation(
                        out=RR[:, sl], in_=PS[pr][:, 0:w], func=AF.Sigmoid, bias=V("lrb", d * 8 + j)),
                        reads=[("PS", pr), "VEC", ("RR", p)], writes=[("RR", p)])
                    k.op("act", lambda e, pi=pi, sl=sl, d=d, j=j, w=w: e.activation(
                        out=II[:, sl], in_=PS[pi][:, 0:w], func=AF.Sigmoid, bias=V("lib", d * 8 + j)),
                        reads=[("PS", pi), "VEC", ("II", p)], writes=[("II", p)])
                    k.op("act", lambda e, d=d, j=j, sl=sl: e.activation(
                        out=XL[:, sl], in_=RR[:, sl], func=AF.Exp, scale=SCL[:, d * 8 + j:d * 8 + j + 1]),
                        reads=[("RR", p), "SCL", ("XLp", p), "XL"], writes=[("XLp", p)])
                    k.op("dve", lambda e, sl=sl: e.tensor_scalar(out=XL[:, sl], in0=XL[:, sl], scalar1=1.0,
                                                                  scalar2=None, op0=ALU.min),
                         reads=[("XLp", p)], writes=[("XLp", p)])
                    k.op("dve", lambda e, sl=sl: e.tensor_tensor(out=RR[:, sl], in0=XL[:, sl], in1=XL[:, sl],
                                                                  op=ALU.mult),
                         reads=[("RR", p), ("XLp", p)], writes=[("RR", p)])

                def stage_y(oi, p, d=d):
                    lo, hi = pieces[p]
                    w = hi - lo
                    sl = slice(lo, hi)
                    k.op("act", lambda e, sl=sl: e.activation(out=RR[:, sl], in_=RR[:, sl], func=AF.Sqrt,
                                                              scale=-1.0, bias=EPSV[:, 1:2]),
                         reads=[("RR", p), "EPSV"], writes=[("RR", p)])
                    k.op("dve", lambda e, sl=sl: e.tensor_tensor(out=II[:, sl], in0=II[:, sl], in1=RR[:, sl],
                                                                  op=ALU.mult),
                         reads=[("RR", p), ("II", p)], writes=[("II", p)])
                    k.op("dve", lambda e, sl=sl: e.tensor_tensor(out=II[:, sl], in0=II[:, sl], in1=XC[:, sl],
                                                                  op=ALU.mult),
                         reads=[("II", p), "XC"], writes=[("II", p)])
                    if d == 0:
                        init = 0.0 if p == 0 else HF[:, lo - 1:lo]
                        prev = [] if p == 0 else [("HFp", p - 1)]
                        k.op("dve", lambda e, sl=sl, init=init: e.tensor_tensor_scan(
                            out=HF[:, sl], data0=XL[:, sl], data1=II[:, sl], initial=init, op0=ALU.mult,
                            op1=ALU.add), reads=[("XLp", p), ("II", p), ("HFp", p), "HF"] + prev,
                            writes=[("HFp", p)])
                    else:
                        if p == 0:
                            init, prev = 0.0, []
                        elif p == NP - 1:
                            init, prev = RR[:, 0:1], [("RR", 0)]
                        else:
                            init, prev = RR[:, hi:hi + 1], [("RR", p + 1)]
                        k.op("dve", lambda e, sl=sl, init=init: e.tensor_tensor_scan(
                            out=RR[:, sl][:, ::-1], data0=XL[:, sl][:, ::-1], data1=II[:, sl][:, ::-1],
                            initial=init, op0=ALU.mult, op1=ALU.add),
                            reads=[("XLp", p), ("II", p), ("RR", p)] + prev, writes=[("RR", p)])
                        k.op("dve", lambda e, sl=sl: e.tensor_tensor(out=HF[:, sl], in0=HF[:, sl], in1=RR[:, sl],
                                                                      op=ALU.add),
                             reads=[("HFp", p), ("RR", p)], writes=[("HFp", p)])

                stage_x(0, order[0])
                for oi, p in enumerate(order):
                    if oi + 1 < NP:
                        stage_x(oi + 1, order[oi + 1])
                    stage_y(oi, p)
            if j + 1 < KC:
                load_bd(j + 1)
            for t in range(NT):
                pb = t % 2
                for kk in range(KC):
                    k.op("pe", lambda e, kk=kk, t=t, pb=pb: e.matmul(
                        PS[pb][:, 0:TW], lhsT=WLG[:, kk, :], rhs=HT[:, kk, t * TW:(t + 1) * TW], start=(kk == 0),
                        stop=(kk == KC - 1)), reads=["WLG", ("HT", t)], writes=[("PS", pb)])
                k.op("act", lambda e, pb=pb: e.activation(out=GE[pb][:], in_=PS[pb][:, 0:TW], func=AF.Gelu),
                     reads=[("PS", pb), ("GE", pb)], writes=[("GE", pb)])
                if t == 0:
                    k.op("dve", lambda e, pb=pb: e.tensor_tensor(out=YB[pb][:], in0=GE[pb][:], in1=HF[:, 0:256],
                                                                  op=ALU.mult),
                         reads=[("GE", pb), ("YB", pb)] + hf_keys, writes=[("YB", pb)])
                else:
                    k.op("dve", lambda e, pb=pb, t=t: e.tensor_tensor(
                        out=YB[pb][:].rearrange("p (r c) -> p r c", c=64),
                        in0=GE[pb][:].rearrange("p (r c) -> p r c", c=64), in1=scan_view(HF, t), op=ALU.mult),
                        reads=[("GE", pb), ("YB", pb)] + hf_keys, writes=[("YB", pb)])
                k.dma("sp", YD[b, DM + j * 128:DM + (j + 1) * 128, t * TW:(t + 1) * TW], YB[pb][:],
                      reads=[("YB", pb)], writes=[("YD", t)])
        k.barrier()
        S.close()

    def phase_C(l, b):
        S = Scope(nc)
        WX = S.sb("WX", [128, KC, 1536], BF16)
        WZ = S.sb("WZ", [128, KC, 1024], BF16)
        WDT = S.sb("WDT", [128, KC, 32], BF16)
        DTB = S.sb("DTB", [128, 32])
        ANEG = S.sb("ANEG", [128, 32])
        DT = S.sb("DT", [128, NCH, 32])
        GT = S.sb("GT", [128, NCH, 32])
        EGT = S.sb("EGT", [128, NCH, 32])
        WTS = S.sb("WTS", [128, NCH, 32])
        ATS = [S.sb("ATS%d" % i, [128, 32]) for i in range(2)]
        TMPA = [S.sb("TMPA%d" % i, [128, 32]) for i in range(2)]
        TMPB = [S.sb("TMPB%d" % i, [128, 32]) for i in range(2)]
        GROWS = [S.sb("GROWS%d" % i, [32, 128]) for i in range(2)]
        HTT = [S.sb("HTT%d" % i, [128, KC, TW + 3], BF16) for i in range(2)]
        RAW = [S.sb("RAW%d" % i, [128, TW + 3]) for i in range(2)]
        ACC = [S.sb("ACC%d" % i, [128, TW]) for i in range(2)]
        XBC = [S.sb("XBC%d" % i, [128, 12, TW], BF16) for i in range(2)]
        SZ = [S.sb("SZ%d" % i, [128, KC, TW], BF16) for i in range(2)]
        TOK = [S.sb("TOK%d" % i, [128, 1280], BF16) for i in range(2)]
        XF = [S.sb("XF%d" % i, [128, 16, 64], BF16) for i in range(2)]
        XB = [S.sb("XB%d" % i, [128, 16, 64], BF16) for i in range(2)]
        XD = S.sb("XD", [128, 16, 64], BF16)
        ACB = S.sb("ACB", [128, 32, 128])
        EB = S.sb("EB", [128, 32, 128])
        WW = [S.sb("WW%d" % i, [128, 32, 128], BF16) for i in range(2)]
        CS = [S.sb("CS%d" % i, [128, 32, 128], BF16) for i in range(2)]
        CBM1 = S.sb("CBM", [128, 4, 128])
        CBM = [CBM1, CBM1]
        YFs = S.sb("YFs", [128, KC, 128])
        YBs = S.sb("YBs", [128, KC, 128], BF16)
        HS = [S.sb("HS%d" % i, [128, 16, 64]) for i in range(2)]
        HSBF = [S.sb("HSBF%d" % i, [128, 16, 64], BF16) for i in range(2)]
        HSBB = [S.sb("HSBB%d" % i, [128, 16, 64], BF16) for i in range(2)]

        load_cast(WX, w_in[l][:, OFF_XBC:OFF_XBC + 1536], KC, 1536, "WX")
        load_cast(WZ, w_in[l][:, 0:1024], KC, 1024, "WZ")
        load_cast(WDT, w_in[l][:, OFF_DT:OFF_DT + 32], KC, 32, "WDT")
        k.dma("sp", DTB[:], dtrow[l, 0:1, :].partition_broadcast(128), writes=["DTB"])
        k.dma("sp", ANEG[:], dtrow[l, 1:2, :].partition_broadcast(128), writes=["ANEG"])
        k.op("act", lambda e: e.activation(out=ANEG[:], in_=ANEG[:], func=AF.Exp), reads=["ANEG"], writes=["ANEG"])
        k.op("dve", lambda e: e.tensor_scalar(out=ANEG[:], in0=ANEG[:], scalar1=-1.0, scalar2=None, op0=ALU.mult),
             reads=["ANEG"], writes=["ANEG"])

        def tok_range(t):
            lo, hi = (0, 256) if t == 0 else (256, T)
            return t * TW, lo, hi

        def load_ht(t, par):
            t0, lo, hi = tok_range(t)
            a, bnd = max(t0 - 2, lo), min(t0 + TW + 1, hi)
            if a > t0 - 2:
                k.op("pool", lambda e: e.memset(HTT[par][:, :, 0:2], 0.0), reads=[("HTT", par)],
                     writes=[("HTT", par)])
            if bnd < t0 + TW + 1:
                k.op("pool", lambda e: e.memset(HTT[par][:, :, TW + 2:TW + 3], 0.0), reads=[("HTT", par)],
                     writes=[("HTT", par)])
            k.dma("sp", HTT[par][:, :, a - (t0 - 2):bnd - (t0 - 2)],
                  HTD[b].rearrange("(c p) t -> p c t", p=128)[:, :, a:bnd], reads=[("HTT", par)],
                  writes=[("HTT", par)])

        def pp1_p(ci):
            t, hf = ci // 2, ci % 2
            par, q = t % 2, ci % 2
            if hf == 0 and t + 1 < NT:
                load_ht(t + 1, (t + 1) % 2)
            c0 = 2 + hf * 128
            pb = 3 * q
            tm, at = TMPA[q], ATS[q]
            for kk in range(KC):
                k.op("pe", lambda e, kk=kk: e.matmul(
                    PS[pb][:, 0:32], lhsT=HTT[par][:, kk, c0:c0 + 128], rhs=WDT[:, kk, :], start=(kk == 0),
                    stop=(kk == KC - 1)), reads=[("HTT", par), "WDT"], writes=[("PS", pb)])
            k.op("dve", lambda e: e.tensor_tensor(out=tm[:], in0=PS[pb][:, 0:32], in1=DTB[:], op=ALU.add),
                 reads=[("PS", pb), "DTB", ("TMPA", q)], writes=[("TMPA", q)])
            k.op("act", lambda e: e.activation(out=tm[:], in_=tm[:], func=AF.Exp), reads=[("TMPA", q)],
                 writes=[("TMPA", q)])
            k.op("act", lambda e: e.activation(out=DT[:, ci, :], in_=tm[:], func=AF.Ln, bias=EPSV[:, 1:2]),
                 reads=[("TMPA", q), "EPSV"], writes=[("DT", ci)])
            k.op("dve", lambda e: e.tensor_tensor(out=at[:], in0=DT[:, ci, :], in1=ANEG[:], op=ALU.mult),
                 reads=[("DT", ci), "ANEG", ("ATS", q)], writes=[("ATS", q)])

        def pp1_q(ci):
            q = ci % 2
            p1, p2 = 1 + 3 * q, 2 + 3 * q
            at, tb = ATS[q], TMPB[q]
            rk = [("ATS", q), "CONST"]
            k.op("pe", lambda e: e.matmul(PS[p1][:, 0:16], lhsT=TRIF, rhs=at[:, 0:16], start=True, stop=True),
                 reads=rk, writes=[("PS", p1)])
            k.op("pe", lambda e: e.matmul(PS[p1][:, 16:32], lhsT=TRIB, rhs=at[:, 16:32], start=True, stop=True),
                 reads=rk, writes=[("PS", p1)])
            k.op("pe", lambda e: e.matmul(PS[p1][:, 32:64], lhsT=ONES, rhs=at[:, :], start=True, stop=True),
                 reads=rk, writes=[("PS", p1)])
            k.op("pe", lambda e: e.matmul(PS[p2][0:32, 0:128], lhsT=at[:, 0:32], rhs=TRIF, start=True, stop=True),
                 reads=rk, writes=[("PS", p2)])
            k.op("pe", lambda e: e.matmul(PS[p2][0:32, 128:256], lhsT=at[:, 0:32], rhs=TRIB, start=True, stop=True),
                 reads=rk, writes=[("PS", p2)])
            k.op("dve", lambda e: e.tensor_copy(out=GT[:, ci, :], in_=PS[p1][:, 0:32]), reads=[("PS", p1)],
                 writes=[("GT", ci)])
            k.op("dve", lambda e: e.tensor_tensor(out=tb[:], in0=PS[p1][:, 32:64], in1=GT[:, ci, :],
                                                  op=ALU.subtract), reads=[("PS", p1), ("GT", ci), ("TMPB", q)],
                 writes=[("TMPB", q), ("P1X", q)])
            k.op("act", lambda e: e.activation(out=EGT[:, ci, :], in_=PS[p1][:, 32:64], func=AF.Exp),
                 reads=[("PS", p1), ("P1X", q)], writes=[("EGT", ci)])
            k.op("act", lambda e: e.activation(out=tb[:], in_=tb[:], func=AF.Exp), reads=[("TMPB", q)],
                 writes=[("TMPB", q)])
            k.op("dve", lambda e: e.tensor_tensor(out=WTS[:, ci, :], in0=tb[:], in1=DT[:, ci, :], op=ALU.mult),
                 reads=[("TMPB", q), ("DT", ci)], writes=[("WTS", ci)])
            k.op("dve", lambda e: e.tensor_scalar(out=GROWS[q][:], in0=PS[p2][0:32, 0:128],
                                                  scalar1=CONST[0:32, 1, 15:16], scalar2=None, op0=ALU.mult),
                 reads=[("PS", p2), ("GROWS", q), "CONST"], writes=[("GROWS", q)])
            k.op("dve", lambda e: e.scalar_tensor_tensor(
                out=GROWS[q][:], in0=PS[p2][0:32, 128:256], scalar=CONST[0:32, 2, 16:17], in1=GROWS[q][:],
                op0=ALU.mult, op1=ALU.add), reads=[("PS", p2), ("GROWS", q), "CONST"], writes=[("GROWS", q)])
            k.dma("sp", GROWD[ci].rearrange("(h l) -> h l", h=32), GROWS[q][:], reads=[("GROWS", q)],
                  writes=["GROWD"])

        load_ht(0, 0)
        pp1_p(0)
        for ci in range(NCH):
            if ci + 1 < NCH:
                pp1_p(ci + 1)
            pp1_q(ci)

        ccn = [0]

        def front(t, par, full):
            load_ht(t, par)
            xb = XBC[par]
            ncc = 12 if full else 10
            pend = []

            def silu_cc(cc, ai):
                k.op("act", lambda e: e.activation(out=xb[:, cc, :], in_=ACC[ai][:], func=AF.Silu),
                     reads=[("ACC", ai)], writes=[("XBC", par, cc)])

            for cc in range(ncc):
                pb = 4 + cc % 2
                ri = ccn[0] % 2
                ai = ccn[0] % 2
                ccn[0] += 1
                for kk in range(KC):
                    k.op("pe", lambda e, kk=kk, cc=cc, pb=pb: e.matmul(
                        PS[pb][:, 0:TW + 3], lhsT=WX[:, kk, cc * 128:(cc + 1) * 128], rhs=HTT[par][:, kk, :],
                        start=(kk == 0), stop=(kk == KC - 1)), reads=["WX", ("HTT", par)], writes=[("PS", pb)])
                k.op("act", lambda e, pb=pb, ri=ri: e.activation(out=RAW[ri][:], in_=PS[pb][:, 0:TW + 3],
                                                                  func=AF.Identity), reads=[("PS", pb), ("RAW", ri)],
                     writes=[("RAW", ri)])
                if pend:
                    silu_cc(*pend.pop(0))
                k.op("dve", lambda e, cc=cc, ri=ri, ai=ai: e.tensor_scalar(
                    out=ACC[ai][:], in0=RAW[ri][:, 2:2 + TW], scalar1=V("scw", 2 * 12 + cc), scalar2=V("scb", cc),
                    op0=ALU.mult, op1=ALU.add), reads=[("RAW", ri), "VEC", ("ACC", ai)], writes=[("ACC", ai)])
                for tap, off in [(0, 0), (1, 1), (3, 3)]:
                    k.op("dve", lambda e, cc=cc, tap=tap, off=off, ri=ri, ai=ai: e.scalar_tensor_tensor(
                        out=ACC[ai][:], in0=RAW[ri][:, off:off + TW], scalar=V("scw", tap * 12 + cc),
                        in1=ACC[ai][:], op0=ALU.mult, op1=ALU.add), reads=[("RAW", ri), ("ACC", ai), "VEC"],
                        writes=[("ACC", ai)])
                pend.append((cc, ai))
            while pend:
                silu_cc(*pend.pop(0))
            if full:
                for cc in range(KC):
                    pb = 4 + cc % 2
                    for kk in range(KC):
                        k.op("pe", lambda e, kk=kk, cc=cc, pb=pb: e.matmul(
                            PS[pb][:, 0:TW], lhsT=WZ[:, kk, cc * 128:(cc + 1) * 128], rhs=HTT[par][:, kk, 2:2 + TW],
                            start=(kk == 0), stop=(kk == KC - 1)), reads=["WZ", ("HTT", par)], writes=[("PS", pb)])
                    k.op("act", lambda e, cc=cc, pb=pb: e.activation(out=SZ[par][:, cc, :], in_=PS[pb][:, 0:TW],
                                                                      func=AF.Silu), reads=[("PS", pb)],
                         writes=[("SZ", par, cc)])

        def to_tok(par, hf, tp):
            for rnd in range(2):
                for i in range(5):
                    cc = rnd * 5 + i
                    k.op("pe", lambda e, cc=cc, i=i: e.transpose(
                        PS[6][:, i * 64:(i + 1) * 64].bitcast(BF16), XBC[par][:, cc, hf * 128:(hf + 1) * 128],
                        IDENTB[:]), reads=[("XBC", par, cc), "IDENTB"], writes=[("PS", 6)])
                if rnd == 0:
                    k.op("dve", lambda e: e.tensor_copy(out=TOK[tp][:, 0:640], in_=PS[6][:, 0:320].bitcast(BF16)),
                         reads=[("PS", 6), ("TOK", tp)], writes=[("TOK", tp)])
                else:
                    k.op("act", lambda e: e.activation(out=TOK[tp][:, 640:1280], in_=PS[6][:, 0:320].bitcast(BF16),
                                                       func=AF.Identity),
                         reads=[("PS", 6), ("TOK", tp)], writes=[("TOK", tp)])

        def state_update(ci, tp, d):
            xt3 = TOK[tp][:, 0:1024].rearrange("p (h q) -> p h q", q=64)
            k.op("dve", lambda e: e.tensor_tensor(
                out=XD[:], in0=xt3, in1=bc(WTS[:, ci, d * 16:(d + 1) * 16].unsqueeze(2), [128, 16, 64]),
                op=ALU.mult), reads=[("TOK", tp), ("WTS", ci), "XD"], writes=["XD"])
            k.op("dve", lambda e: e.tensor_tensor(
                out=HS[d][:], in0=HS[d][:], in1=bc(EGT[:, ci, d * 16:(d + 1) * 16].unsqueeze(2), [128, 16, 64]),
                op=ALU.mult), reads=[("HS", d), ("EGT", ci)], writes=[("HS", d)])
            for g in range(2):
                k.op("pe", lambda e, g=g: e.matmul(
                    PS[7][:, 0:512], lhsT=TOK[tp][:, 1024 + g * 128:1024 + (g + 1) * 128],
                    rhs=XD[:, 8 * g:8 * g + 8, :].rearrange("p h q -> p (h q)"), start=True, stop=True),
                    reads=[("TOK", tp), "XD"], writes=[("PS", 7)])
                k.op("dve", lambda e, g=g: e.tensor_tensor(
                    out=HS[d][:, 8 * g:8 * g + 8, :], in0=HS[d][:, 8 * g:8 * g + 8, :],
                    in1=PS[7][:, 0:512].rearrange("p (h q) -> p h q", q=64), op=ALU.add),
                    reads=[("HS", d), ("PS", 7)], writes=[("HS", d)])

        all_xbc = lambda par, lo, hi: [("XBC", par, c) for c in range(lo, hi)]

        k.op("pool", lambda e: e.memset(HS[1][:], 0.0), writes=[("HS", 1)])
        k.op("pool", lambda e: e.memset(HS[0][:], 0.0), writes=[("HS", 0)])
        order = [0] + list(range(NT - 1, 0, -1))
        front(order[0], 0, False)
        for it, t in enumerate(order):
            par = it % 2
            if it + 1 < len(order):
                front(order[it + 1], (it + 1) % 2, False)
            for hf in (1, 0):
                ci = 2 * t + hf
                tp = hf
                to_tok(par, hf, tp)
                hb = HSBB[ci % 2]
                k.op("act", lambda e, hb=hb: e.activation(out=hb[:], in_=HS[1][:], func=AF.Identity),
                     reads=[("HS", 1), ("HSBB", ci % 2)], writes=[("HSBB", ci % 2)])
                k.dma("sp", HBD[ci], hb[:].rearrange("p h q -> p (h q)"), reads=[("HSBB", ci % 2)],
                      writes=[("HBD", ci)])
                state_update(ci, tp, 1)

        def stage_a(ci):
            t, hf = ci // 2, ci % 2
            par, tp, cp = t % 2, hf, ci % 2
            sl = slice(hf * 128, (hf + 1) * 128)
            xb = XBC[par]
            to_tok(par, hf, tp)
            k.dma("sp", ACB[:].rearrange("p h l -> p (h l)"), GROWD[ci:ci + 1, :].partition_broadcast(128),
                  reads=["GROWD", "ACB"], writes=["ACB"])
            k.dma("sp", HSBB[cp][:].rearrange("p h q -> p (h q)"), HBD[ci], reads=[("HBD", ci), ("HSBB", cp)],
                  writes=[("HSBB", cp)])
            k.op("act", lambda e: e.activation(out=HSBF[cp][:], in_=HS[0][:], func=AF.Identity),
                 reads=[("HS", 0), ("HSBF", cp)], writes=[("HSBF", cp)])
            state_update(ci, tp, 0)
            xt3 = TOK[tp][:, 0:1024].rearrange("p (h q) -> p h q", q=64)
            k.op("dve", lambda e: e.tensor_tensor(
                out=XF[cp][:], in0=xt3, in1=bc(DT[:, ci, 0:16].unsqueeze(2), [128, 16, 64]), op=ALU.mult),
                reads=[("TOK", tp), ("DT", ci), ("XF", cp)], writes=[("XF", cp)])
            k.op("pool", lambda e: e.tensor_tensor(
                out=XB[cp][:], in0=xt3, in1=bc(DT[:, ci, 16:32].unsqueeze(2), [128, 16, 64]), op=ALU.mult),
                reads=[("TOK", tp), ("DT", ci), ("XB", cp)], writes=[("XB", cp)])
            for g in range(2):
                k.op("pe", lambda e, g=g: e.matmul(
                    PS[6][:, g * 128:(g + 1) * 128], lhsT=xb[:, 8 + g, sl], rhs=xb[:, 10 + g, sl],
                    start=True, stop=True), reads=all_xbc(par, 8, 12), writes=[("PS", 6)])
            for d, tri in enumerate((TRIF, TRIB)):
                k.op("dve", lambda e, d=d, tri=tri: e.tensor_tensor(
                    out=CBM[cp][:, 2 * d:2 * d + 2, :], in0=PS[6][:, 0:256].rearrange("p (g l) -> p g l", g=2),
                    in1=bc(tri.unsqueeze(1), [128, 2, 128]), op=ALU.mult),
                    reads=[("PS", 6), "CONST", "CBM"], writes=["CBM"])
            k.op("act", lambda e: e.activation(out=EB[:], in_=ACB[:], func=AF.Exp), reads=["ACB", "EB"],
                 writes=["EB"])
            for d in range(2):
                k.op("pool", lambda e, d=d: e.tensor_tensor(
                    out=CS[cp][:, 16 * d:16 * d + 16, :].rearrange("p (g h) l -> p g h l", g=2),
                    in0=EB[:, 16 * d:16 * d + 16, :].rearrange("p (g h) l -> p g h l", g=2),
                    in1=bc(xb[:, 10:12, sl].unsqueeze(2), [128, 2, 8, 128]), op=ALU.mult),
                    reads=["EB", ("CS", cp)] + all_xbc(par, 10, 12), writes=[("CS", cp)])
            k.op("dve", lambda e: e.tensor_tensor(
                out=ACB[:], in0=ACB[:], in1=bc(GT[:, ci, :].unsqueeze(2), [128, 32, 128]), op=ALU.subtract),
                reads=["ACB", ("GT", ci), "EB"], writes=["ACB"])
            k.op("dve", lambda e: e.tensor_scalar(out=ACB[:], in0=ACB[:], scalar1=0.0, scalar2=None,
                                                  op0=ALU.min), reads=["ACB"], writes=["ACB"])
            k.op("act", lambda e: e.activation(out=ACB[:], in_=ACB[:], func=AF.Exp), reads=["ACB"],
                 writes=["ACB"])
            for d in range(2):
                k.op("dve", lambda e, d=d: e.tensor_tensor(
                    out=WW[cp][:, 16 * d:16 * d + 16, :].rearrange("p (g h) l -> p g h l", g=2),
                    in0=ACB[:, 16 * d:16 * d + 16, :].rearrange("p (g h) l -> p g h l", g=2),
                    in1=bc(CBM[cp][:, 2 * d:2 * d + 2, :].unsqueeze(2), [128, 2, 8, 128]), op=ALU.mult),
                    reads=["ACB", "CBM", ("WW", cp)], writes=[("WW", cp)])

        def stage_b(ci):
            t, hf = ci // 2, ci % 2
            par, tp, cp = t % 2, hf, ci % 2
            sl = slice(hf * 128, (hf + 1) * 128)
            xb = XBC[par]
            for h in range(16):
                po = 64 * (h % 2)
                cc = h // 2
                pb = 2 * cp + cc // 4
                ov = PS[pb][po:po + 64, (cc % 4) * 128:(cc % 4 + 1) * 128]
                ops = [(XF[cp][:, h, :], WW[cp][:, h, :], [("XF", cp), ("WW", cp)]),
                       (XB[cp][:, h, :], WW[cp][:, 16 + h, :], [("XB", cp), ("WW", cp)]),
                       (HSBF[cp][:, h, :], CS[cp][:, h, :], [("HSBF", cp), ("CS", cp)]),
                       (HSBB[cp][:, h, :], CS[cp][:, 16 + h, :], [("HSBB", cp), ("CS", cp)])]
                for i, (lt, rh, rk) in enumerate(ops):
                    k.op("pe", lambda e, ov=ov, lt=lt, rh=rh, i=i: e.matmul(ov, lhsT=lt, rhs=rh, start=(i == 0),
                                                                               stop=(i == 3)),
                         reads=rk, writes=[("PS", pb)])
            for cc in range(KC):
                pb = 2 * cp + cc // 4
                pv = PS[pb][:, (cc % 4) * 128:(cc % 4 + 1) * 128]
                k.op("dve", lambda e, cc=cc, pv=pv: e.scalar_tensor_tensor(
                    out=YFs[:, cc, :], in0=xb[:, cc, sl], scalar=V("sdv", cc), in1=pv, op0=ALU.mult,
                    op1=ALU.add), reads=[("XBC", par, cc), ("PS", pb), "VEC", "YFs"], writes=["YFs"])
            k.op("dve", lambda e: e.tensor_tensor(out=YBs[:], in0=YFs[:], in1=SZ[par][:, :, sl], op=ALU.mult),
                 reads=["YFs", "YBs"] + [("SZ", par, c) for c in range(KC)], writes=["YBs"])
            k.dma("sp", YD[b, 0:DM, ci * 128:(ci + 1) * 128].rearrange("(c p) t -> p c t", p=128), YBs[:],
                  reads=["YBs"], writes=[("YDs", ci)])

        front(0, 0, True)
        stage_a(0)
        for ci in range(NCH):
            nx = ci + 1
            if nx < NCH:
                if nx % 2 == 0:
                    front(nx // 2, (nx // 2) % 2, True)
                stage_a(nx)
            stage_b(ci)
        k.barrier()
        S.close()

    def phase_D(l, b, last):
        S = Scope(nc)
        WO = S.sb("WO", [128, 16, DM], BF16)
        YT = [S.sb("YT%d" % i, [128, 16, TW], BF16) for i in range(2)]
        XT = [S.sb("XTd%d" % i, [128, KC, TW]) for i in range(2)]
        RB = [S.sb("RB%d" % i, [128, 2, TW]) for i in range(2)]
        T1 = [S.sb("T1_%d" % i, [128, TW]) for i in range(2)]
        T2 = [S.sb("T2_%d" % i, [128, TW]) for i in range(2)]
        SQd = S.sb("SQd", [128, 16, TW])
        load_cast(WO, w_out[l], 16, DM, "WO", scale_vec=lambda kk: V("snw", kk) if kk < 8 else V("lnw", kk - 8))
        src = xsrc_of(l)
        tl = list(range(1 if last else 0, NT))

        def loads(t):
            par = t % 2
            sl = slice(t * TW, (t + 1) * TW)
            k.dma("sp", YT[par][:], YD[b].rearrange("(c p) t -> p c t", p=128)[:, :, sl], writes=[("YT", par)])
            k.dma("sp", XT[par][:], xtile(src, b, t), writes=[("XTd", par)])

        loads(tl[0])
        for ti, t in enumerate(tl):
            par = t % 2
            col = 2 if t == 0 else b
            if ti + 1 < len(tl):
                loads(tl[ti + 1])
            k.op("act", lambda e, par=par: e.activation(out=SQd[:], in_=YT[par][:], func=AF.Square),
                 reads=[("YT", par), "SQd"], writes=["SQd"])
            for half in range(2):
                for kk in range(8):
                    k.op("pe", lambda e, kk=kk, half=half: e.matmul(
                        PS[2][:, half * TW:(half + 1) * TW], lhsT=ONES, rhs=SQd[:, half * 8 + kk, :],
                        start=(kk == 0), stop=(kk == 7)), reads=["SQd", "CONST"], writes=[("PS", 2)])
            rstd_from_ms(RB[par][:].rearrange("p a t -> p (a t)"), PS[2][:, 0:2 * TW], [("PS", 2)], [("RB", par)])
            for d in range(KC):
                pb = d % 2
                t1, t2 = T1[pb], T2[pb]
                for kk in range(16):
                    half = kk // 8
                    k.op("pe", lambda e, kk=kk, d=d, pb=pb, half=half: e.matmul(
                        PS[pb][:, half * TW:(half + 1) * TW], lhsT=WO[:, kk, d * 128:(d + 1) * 128],
                        rhs=YT[par][:, kk, :], start=(kk % 8 == 0), stop=(kk % 8 == 7)),
                        reads=["WO", ("YT", par)], writes=[("PS", pb)])
                k.op("dve", lambda e, pb=pb, t1=t1: e.tensor_tensor(out=t1[:], in0=PS[pb][:, 0:TW],
                                                                     in1=RB[par][:, 0, :], op=ALU.mult),
                     reads=[("PS", pb), ("RB", par), ("T1", pb)], writes=[("T1", pb)])
                k.op("dve", lambda e, pb=pb, t2=t2: e.tensor_tensor(out=t2[:], in0=PS[pb][:, TW:2 * TW],
                                                                     in1=RB[par][:, 1, :], op=ALU.mult),
                     reads=[("PS", pb), ("RB", par), ("T2", pb)], writes=[("T2", pb)])
                k.op("dve", lambda e, t1=t1, t2=t2: e.tensor_tensor(out=t1[:], in0=t1[:], in1=t2[:], op=ALU.add),
                     reads=[("T1", pb), ("T2", pb)], writes=[("T1", pb)])
                k.op("dve", lambda e, d=d, t1=t1: e.scalar_tensor_tensor(
                    out=XT[par][:, d, :], in0=t1[:], scalar=MODP[:, 2, d, col:col + 1], in1=XT[par][:, d, :],
                    op0=ALU.mult, op1=ALU.add), reads=[("T1", pb), ("XTd", par), "MODP"], writes=[("XTd", par)])
            k.dma("sp", xtile(XR, b, t), XT[par][:], reads=[("XTd", par)], writes=[("XR", b, t)])
        k.barrier()
        S.close()

    def phase_F(l, last):
        moe = (l % 2 == 1)
        li = l // 2
        tiles = [(b, t) for b in range(NB) for t in range(1 if last else 0, NT)]
        S = Scope(nc)
        st = {n: [S.sb("%sf%d" % (n, i), [128, KC, TW]) for i in range(2)] for n in ("XT", "SQ", "XN")}
        st["RS"] = [S.sb("RSf%d" % i, [128, TW]) for i in range(2)]
        HN = [S.sb("HN%d" % i, [128, KC, TW]) for i in range(2)]
        HNB = [S.sb("HNB%d" % i, [128, KC, TW], BF16) for i in range(2)]
        RW = S.sb("RW", [128, KC, NE])
        LG = S.sb("LG", [128, 8, NE])
        SM = S.sb("SM", [128, 8])
        CT = S.sb("CT", [NE, 128])
        if moe:
            k.dma("sp", RW[:], router_w[li].rearrange("(k p) n -> p k n", p=128), writes=["RW"])
        def load(it):
            b, t = tiles[it]
            k.dma("sp", st["XT"][it % 2][:], xtile(XR, b, t), writes=[("XT", it % 2)])

        def stage2(it):
            b, t = tiles[it]
            par = it % 2
            col = 2 if t == 0 else b
            if moe:
                norm_s2(st, par, 3, col, lambda c, par=par: (HN[par][:, c, :], [("HN", par)]),
                        extra_reads=[("HN", par)])
                k.op("dve", lambda e, par=par: e.tensor_copy(out=HNB[par][:], in_=HN[par][:]),
                     reads=[("HN", par), ("HNB", par)], writes=[("HNB", par)])
            else:
                norm_s2(st, par, 3, col, lambda c, par=par: (HNB[par][:, c, :], [("HNB", par)]),
                        extra_reads=[("HNB", par)])
            k.dma("sp", H2D.rearrange("(c p) t -> p c t", p=128)[:, :, b * T + t * TW:b * T + (t + 1) * TW],
                  HNB[par][:], reads=[("HNB", par)], writes=["H2D"])
            if moe:
                for hf in range(2):
                    for c in range(KC):
                        k.op("pe", lambda e, c=c, hf=hf, par=par: e.matmul(
                            PS[2][:, 0:NE], lhsT=HN[par][:, c, hf * 128:(hf + 1) * 128], rhs=RW[:, c, :],
                            start=(c == 0), stop=(c == KC - 1)), reads=[("HN", par), "RW"], writes=[("PS", 2)])
                    L0, EQ1, L2, EQ2, CB_ = (LG[:, i, :] for i in range(5))
                    M1, M2, DD, P1, P2 = (SM[:, i:i + 1] for i in range(5))
                    seq = [
                        ("dve", lambda e: e.tensor_copy(out=L0, in_=PS[2][:, 0:NE]), [("PS", 2)]),
                        ("dve", lambda e: e.tensor_reduce(out=M1, in_=L0, axis=AX.X, op=ALU.max), []),
                        ("dve", lambda e: e.tensor_scalar(out=EQ1, in0=L0, scalar1=M1, scalar2=None,
                                                          op0=ALU.is_equal), []),
                        ("dve", lambda e: e.scalar_tensor_tensor(out=L2, in0=EQ1, scalar=-1e30, in1=L0,
                                                                 op0=ALU.mult, op1=ALU.add), []),
                        ("dve", lambda e: e.tensor_reduce(out=M2, in_=L2, axis=AX.X, op=ALU.max), []),
                        ("dve", lambda e: e.tensor_scalar(out=EQ2, in0=L2, scalar1=M2, scalar2=None,
                                                          op0=ALU.is_equal), []),
                        ("dve", lambda e: e.tensor_tensor(out=DD, in0=M2, in1=M1, op=ALU.subtract), []),
                        ("act", lambda e: e.activation(out=DD, in_=DD, func=AF.Exp), []),
                        ("dve", lambda e: e.tensor_scalar(out=P1, in0=DD, scalar1=1.0, scalar2=None, op0=ALU.add),
                         []),
                        ("dve", lambda e: e.reciprocal(out=P1, in_=P1), []),
                        ("dve", lambda e: e.tensor_tensor(out=P2, in0=DD, in1=P1, op=ALU.mult), []),
                        ("dve", lambda e: e.tensor_scalar(out=CB_, in0=EQ1, scalar1=P1, scalar2=None,
                                                          op0=ALU.mult), []),
                        ("dve", lambda e: e.scalar_tensor_tensor(out=CB_, in0=EQ2, scalar=P2, in1=CB_,
                                                                 op0=ALU.mult, op1=ALU.add), []),
                    ]
                    for eng, fn, rk in seq:
                        k.op(eng, fn, reads=["LG"] + rk, writes=["LG"])
                    k.op("pe", lambda e: e.transpose(PS[3][0:NE, 0:128], CB_, IDENTF), reads=["LG", "CONST"],
                         writes=[("PS", 3)])
                    k.op("dve", lambda e: e.tensor_copy(out=CT[:], in_=PS[3][0:NE, 0:128]),
                         reads=[("PS", 3), "CT"], writes=["CT"])
                    o0 = b * T + t * TW + hf * 128
                    k.dma("sp", CWD[:, o0:o0 + 128], CT[:], reads=["CT"], writes=["CWD"])

        norm_pipeline(st, len(tiles), load, stage2)
        k.barrier()
        S.close()
        S = Scope(nc)
        W1 = S.sb("W1", [128, KC, DFF], BF16)
        W3 = S.sb("W3", [128, KC, DFF], BF16)
        W2 = S.sb("W2", [128, FC, DM], BF16)
        H2T = [S.sb("H2T%d" % i, [128, KC, TW], BF16) for i in range(2)]
        XT = [S.sb("XTe%d" % i, [128, KC, TW]) for i in range(2)]
        CWB = [S.sb("CWB%d" % i, [128, TW]) for i in range(2)]
        SG = [S.sb("SG%d" % i, [128, TW]) for i in range(2)]
        AV = S.sb("AVall", [128, FC, TW], BF16)
        T1 = [S.sb("T1e%d" % i, [128, TW]) for i in range(2)]
        stg_extra.extend([S.sb("STGF%d" % i, [128, 1024]) for i in range(4)])
        nex = NE if moe else 1
        WG = 704

        def wgroups(f):
            return sorted({(f * 128) // WG, ((f + 1) * 128 - 1) // WG})

        def load_weights(ex):
            if moe:
                s1, s3, s2 = moe_w1[li, ex], moe_w3[li, ex], moe_w2[li, ex]
            else:
                s1, s3, s2 = ffn_w1[li], ffn_w3[li], ffn_w2[li]
            load_cast(W1, s1, KC, DFF, lambda kk, c0: ("W1", c0 // WG))
            load_cast(W3, s3, KC, DFF, lambda kk, c0: ("W3", c0 // WG))
            load_cast(W2, s2, FC, DM, lambda kk, c0: ("W2", kk))

        load_weights(0)
        for ex in range(nex):
            def loads(it, ex=ex):
                b, t = tiles[it]
                par = it % 2
                o0 = b * T + t * TW
                k.dma("sp", H2T[par][:], H2D.rearrange("(c p) t -> p c t", p=128)[:, :, o0:o0 + TW],
                      reads=["H2D"], writes=[("H2T", par)])
                k.dma("sp", XT[par][:], xtile(XR, b, t), reads=[("XR", b, t)], writes=[("XTe", par)])
                if moe:
                    k.dma("sp", CWB[par][:], CWD[ex:ex + 1, o0:o0 + TW].partition_broadcast(128), reads=["CWD"],
                          writes=[("CWB", par)])

            loads(0)
            for it, (b, t) in enumerate(tiles):
                par = it % 2
                col = 2 if t == 0 else b
                o0 = b * T + t * TW
                if it + 1 < len(tiles):
                    loads(it + 1)
                for f in range(FC):
                    fp = f % 2
                    pg, pu = 4 + fp, 6 + fp
                    for kk in range(KC):
                        k.op("pe", lambda e, kk=kk, f=f, pg=pg: e.matmul(
                            PS[pg][:, 0:TW], lhsT=W1[:, kk, f * 128:(f + 1) * 128], rhs=H2T[par][:, kk, :],
                            start=(kk == 0), stop=(kk == KC - 1)),
                            reads=[("W1", g) for g in wgroups(f)] + [("H2T", par)], writes=[("PS", pg)])
                    for kk in range(KC):
                        k.op("pe", lambda e, kk=kk, f=f, pu=pu: e.matmul(
                            PS[pu][:, 0:TW], lhsT=W3[:, kk, f * 128:(f + 1) * 128], rhs=H2T[par][:, kk, :],
                            start=(kk == 0), stop=(kk == KC - 1)),
                            reads=[("W3", g) for g in wgroups(f)] + [("H2T", par)], writes=[("PS", pu)])
                    k.op("act", lambda e, fp=fp, pg=pg: e.activation(out=SG[fp][:], in_=PS[pg][:, 0:TW],
                                                                      func=AF.Silu), reads=[("PS", pg)],
                         writes=[("SG", fp)])
                    k.op("dve", lambda e, fp=fp, pu=pu, f=f: e.tensor_tensor(out=AV[:, f, :], in0=SG[fp][:],
                                                                              in1=PS[pu][:, 0:TW], op=ALU.mult),
                         reads=[("SG", fp), ("PS", pu)], writes=[("AV", f)])
                for d in range(KC):
                    dp = d % 2
                    for f in range(FC):
                        k.op("pe", lambda e, d=d, f=f, dp=dp: e.matmul(
                            PS[dp][:, 0:TW], lhsT=W2[:, f, d * 128:(d + 1) * 128], rhs=AV[:, f, :],
                            start=(f == 0), stop=(f == FC - 1)), reads=[("W2", f), ("AV", f)],
                            writes=[("PS", dp)])
                    pv = PS[dp][:, 0:TW]
                    if moe:
                        k.op("dve", lambda e, pv=pv, dp=dp: e.tensor_tensor(out=T1[dp][:], in0=pv, in1=CWB[par][:],
                                                                             op=ALU.mult),
                             reads=[("PS", dp), ("CWB", par), ("T1e", dp)], writes=[("T1e", dp)])
                        src_ap, rk = T1[dp][:], [("T1e", dp)]
                    else:
                        src_ap, rk = pv, [("PS", dp)]
                    k.op("dve", lambda e, d=d, src_ap=src_ap: e.scalar_tensor_tensor(
                        out=XT[par][:, d, :], in0=src_ap, scalar=MODP[:, 5, d, col:col + 1], in1=XT[par][:, d, :],
                        op0=ALU.mult, op1=ALU.add), reads=rk + [("XTe", par), "MODP"], writes=[("XTe", par)])
                if it == len(tiles) - 1 and ex + 1 < nex:
                    load_weights(ex + 1)
                k.dma("sp", xtile(XR, b, t), XT[par][:], reads=[("XTe", par)], writes=[("XR", b, t)])
        k.barrier()
        del stg_extra[:]
        S.close()

    def phase_out():
        S = Scope(nc)
        st = {n: [S.sb("%so%d" % (n, i), [128, KC, TW]) for i in range(2)] for n in ("XT", "SQ", "XN")}
        st["RS"] = [S.sb("RSo%d" % i, [128, TW]) for i in range(2)]
        tiles = [(b, t) for b in range(NB) for t in range(1, NT)]

        def load(it):
            b, t = tiles[it]
            k.dma("sp", st["XT"][it % 2][:], xtile(XR, b, t), writes=[("XT", it % 2)])

        def stage2(it):
            b, t = tiles[it]
            par = it % 2
            XN = st["XN"][par]
            k.op("dve", lambda e: e.tensor_tensor(out=XN[:], in0=XN[:], in1=bc(V("fnw", 0, 8).unsqueeze(2),
                                                                                [128, KC, TW]), op=ALU.mult),
                 reads=[("XN", par), "VEC"], writes=[("XN", par)])
            k.dma("sp", out[b].rearrange("(c p) t -> p c t", p=128)[:, :, (t - 1) * TW:t * TW], XN[:],
                  reads=[("XN", par)], writes=["OUT"])

        norm_pipeline(st, len(tiles), load, stage2)
        k.barrier()
        S.close()

    for l in range(nlayers):
        last = (l == 1)
        layer_setup(l)
        k.barrier()
        for b in range(NB):
            if "A" in phases:
                SH = Scope(nc)
                HT = SH.sb("HT", [128, KC, T], BF16)
                phase_A(l, b, HT)
                if "B" in phases:
                    phase_B(l, b, HT)
                k.barrier()
                SH.close()
            if "C" in phases:
                phase_C(l, b)
            if "D" in phases:
                phase_D(l, b, last)
        if "F" in phases:
            phase_F(l, last)
    if nlayers == 2 and "F" in phases:
        phase_out()
    k.barrier()
    return nc, k


def _pack_vecs(inp, l):
    def fm(v):
        v = np.asarray(v, np.float32)
        return v.reshape(-1, 128).T

    cols = [fm(inp["norm1_w"][l]), fm(inp["norm2_w"][l]), fm(inp["mod_b"][l])]
    cols += [fm(inp["ssd_conv_w"][l][tap]) for tap in range(4)]
    cols += [fm(inp["ssd_conv_b"][l]), fm(inp["ssd_norm_w"][l])]
    cols += [fm(inp["lru_conv_w"][l][tap]) for tap in range(4)]
    cols += [fm(inp["lru_conv_b"][l])]
    cols += [fm(inp["lru_rb"][l][d]) for d in range(2)]
    cols += [fm(inp["lru_ib"][l][d]) for d in range(2)]
    cols += [fm(inp["lru_lambda"][l][d]) for d in range(2)]
    cols += [fm(inp["lru_norm_w"][l]), fm(np.repeat(np.asarray(inp["ssd_d"][l], np.float32), 64)),
             fm(inp["final_norm_w"])]
    out = np.concatenate(cols, axis=1)
    assert out.shape == (128, NV), out.shape
    return out


def _consts():
    c = np.zeros((128, 4, 128), np.float32)
    c[:, 0, :] = 1.0
    kk = np.arange(128)[:, None]
    ll = np.arange(128)[None, :]
    c[:, 1, :] = (kk <= ll)
    c[:, 2, :] = (kk >= ll)
    c[:, 3, :] = (kk == ll)
    return c


def make_in_maps(inp, cores=range(NCORES), lite=False):
    f = lambda a: np.ascontiguousarray(np.asarray(a, dtype=np.float32))
    vecs = np.stack([_pack_vecs(inp, l) for l in range(2)], 0)
    dtrow = np.stack([np.stack([np.asarray(inp["ssd_dt_bias"][l]).reshape(32),
                                np.asarray(inp["ssd_a_log"][l]).reshape(32)], 0) for l in range(2)], 0)
    shared = {"vecs": f(vecs), "consts": _consts(), "dtrow": f(dtrow)}
    for n in ("mod_w", "w_in", "lru_rw", "lru_iw", "w_out", "ffn_w1", "ffn_w3", "ffn_w2", "router_w", "moe_w1",
              "moe_w3", "moe_w2"):
        if lite and n.startswith("moe_w"):
            continue
        shared[n] = f(inp[n])
    maps = []
    for i in cores:
        bs = [NB * i + j for j in range(NB)]
        xin = np.stack([np.concatenate([inp["ctx"][b], inp["x"][b]], 0).T for b in bs], 0)
        cv = np.zeros((128, KC, 4), np.float32)
        for j, b in enumerate(bs):
            cv[:, :, j] = np.asarray(inp["c"][b], np.float32).reshape(KC, 128).T
        cv[:, :, 2] = np.asarray(inp["c_ctx"], np.float32).reshape(KC, 128).T
        m = dict(shared)
        m["xin"] = f(xin)
        m["cvec"] = cv
        maps.append(m)
    return maps


def kernel(**inputs):
    nc, _ = build()
    maps = make_in_maps(inputs)
    res = run_bass_kernel_spmd(nc, maps, core_ids=list(range(NCORES)))
    outs = []
    for i in range(NCORES):
        o = np.asarray(res.results[i]["out"])
        outs.append(np.transpose(o, (0, 2, 1)))
    return np.ascontiguousarray(np.concatenate(outs, 0).astype(np.float32))
```
